# Optimizing a Trainium2 kernel written in Bass

```python
import jax, jax.numpy as jnp
from jax import lax
import numpy as np

D_MODEL = 1024
BATCH = 8
SEQ = 4096
DEPTH = 1

MEM_LEN = 256
CONV_WIDTH = D_MODEL // 2
CONV_K = 3
ATT_HEADS = 8
HEAD_DIM = 64
ATT_WIDTH = ATT_HEADS * HEAD_DIM
KV_GROUPS = 2
Q_PER_KV = ATT_HEADS // KV_GROUPS
KV_WIDTH = KV_GROUPS * HEAD_DIM
MIX_WIDTH = CONV_WIDTH + ATT_WIDTH
CMP_BLOCK = 32
CMP_STRIDE = 16
CMP_HIDDEN = 256
SLC_BLOCK = 64
N_SELECT = 16
WINDOW = 512
Q_BLOCK = 128
ROPE_THETA = 10000.0
X_HEADS = 4
X_HEAD_DIM = D_MODEL // X_HEADS
D_FF = -(-8 * D_MODEL // (3 * 256)) * 256
EPS = 1e-6
FORCE = 1e4
W_IN_COLS = 3 * CONV_WIDTH + ATT_WIDTH + 6 * KV_WIDTH + 3 * ATT_HEADS

kernel_name = "hymba_conv_nsa_sandwich_block"


def rmsnorm(x, g):
    xf = x.astype(jnp.float32)
    y = xf * lax.rsqrt(jnp.mean(xf * xf, axis=-1, keepdims=True) + EPS)
    return (y * g.astype(jnp.float32)).astype(x.dtype)


def rope(x, pos):
    half = HEAD_DIM // 2
    inv = ROPE_THETA ** (-jnp.arange(half, dtype=jnp.float32) / half)
    ang = pos.astype(jnp.float32)[..., None] * inv
    cos = jnp.cos(ang)[:, :, None, :]
    sin = jnp.sin(ang)[:, :, None, :]
    xf = x.astype(jnp.float32)
    x1, x2 = xf[..., :half], xf[..., half:]
    return jnp.concatenate([x1 * cos - x2 * sin, x2 * cos + x1 * sin], axis=-1).astype(x.dtype)


def masked_softmax(s, mask):
    s = jnp.where(mask, s.astype(jnp.float32), -1e30)
    e = jnp.exp(s - jnp.max(s, axis=-1, keepdims=True)) * mask
    return e / jnp.maximum(jnp.sum(e, axis=-1, keepdims=True), 1e-30)


def compress(k, pe, w1, w2, n_cmp):
    b = k.shape[0]
    idx = np.arange(n_cmp)[:, None] * CMP_STRIDE + np.arange(CMP_BLOCK)[None, :]
    blocks = k[:, idx] + pe[None, None, :, None, :]
    flat = blocks.transpose(0, 1, 3, 2, 4).reshape(b, n_cmp, KV_GROUPS, CMP_BLOCK * HEAD_DIM)
    return jax.nn.silu(flat @ w1) @ w2


def short_conv_mixer(bg, cg, xin, conv_w):
    z = cg * xin
    zc = lax.conv_general_dilated(z, conv_w[:, None, :].astype(z.dtype), window_strides=(1,),
                                  padding=((CONV_K - 1, 0),), dimension_numbers=("NWC", "WIO", "NWC"),
                                  feature_group_count=CONV_WIDTH)
    return bg * zc


def nsa_mixer(q, k_cmp_raw, v_cmp_raw, k_slc, v_slc, k_win, v_win, gates, positions,
              pe_kc, w1_kc, w2_kc, pe_vc, w1_vc, w2_vc):
    b, s = q.shape[0], q.shape[1]
    n_cmp = (s - CMP_BLOCK) // CMP_STRIDE + 1
    n_slc = s // SLC_BLOCK
    n_sel = min(N_SELECT, n_slc)
    scale = HEAD_DIM ** -0.5

    q = rope(q, positions)
    cmp_end_np = np.arange(n_cmp) * CMP_STRIDE + CMP_BLOCK - 1
    cmp_end = jnp.asarray(cmp_end_np, dtype=jnp.int32)
    k_cmp = rope(compress(k_cmp_raw, pe_kc, w1_kc, w2_kc, n_cmp), positions[:, cmp_end_np])
    v_cmp = compress(v_cmp_raw, pe_vc, w1_vc, w2_vc, n_cmp)

    ci = np.arange(n_cmp)[:, None] * CMP_STRIDE
    sj = np.arange(n_slc)[None, :] * SLC_BLOCK
    ov = np.clip(np.minimum(ci + CMP_BLOCK, sj + SLC_BLOCK) - np.maximum(ci, sj), 0, None)
    slc_map = jnp.asarray(ov / CMP_BLOCK, dtype=jnp.float32)

    k_blocks = rope(k_slc, positions).reshape(b, n_slc, SLC_BLOCK, KV_GROUPS, HEAD_DIM).transpose(0, 3, 1, 2, 4)
    v_blocks = v_slc.reshape(b, n_slc, SLC_BLOCK, KV_GROUPS, HEAD_DIM).transpose(0, 3, 1, 2, 4)
    pad = ((0, 0), (WINDOW, 0), (0, 0), (0, 0))
    k_win_pad = jnp.pad(rope(k_win, positions), pad)
    v_win_pad = jnp.pad(v_win, pad)
    bi = jnp.arange(b)[:, None, None, None]
    gi = jnp.arange(KV_GROUPS)[None, :, None, None]
    blk = jnp.arange(n_slc)

    def block(i):
        s0 = i * Q_BLOCK
        t = s0 + jnp.arange(Q_BLOCK)
        qb = lax.dynamic_slice_in_dim(q, s0, Q_BLOCK, axis=1).reshape(b, Q_BLOCK, KV_GROUPS, Q_PER_KV, HEAD_DIM)
        gb = lax.dynamic_slice_in_dim(gates, s0, Q_BLOCK, axis=1).reshape(b, Q_BLOCK, KV_GROUPS, Q_PER_KV, 3)

        s_c = jnp.einsum("bqgrd,bkgd->bgrqk", qb, k_cmp) * scale
        p_c = masked_softmax(s_c, cmp_end[None, :] <= t[:, None])
        o_c = jnp.einsum("bgrqk,bkgd->bqgrd", p_c, v_cmp)

        imp = jnp.einsum("bgrqk,ks->bgqs", p_c, slc_map)
        cur = t // SLC_BLOCK
        forced = (blk[None, :] == 0) | (blk[None, :] == cur[:, None]) | (blk[None, :] == cur[:, None] - 1)
        future = blk[None, :] * SLC_BLOCK > t[:, None]
        imp = jnp.where(forced, FORCE, jnp.where(future, -FORCE, imp))
        _, sel = lax.top_k(imp, n_sel)
        ks = k_blocks[bi, gi, sel].reshape(b, KV_GROUPS, Q_BLOCK, n_sel * SLC_BLOCK, HEAD_DIM)
        vs = v_blocks[bi, gi, sel].reshape(b, KV_GROUPS, Q_BLOCK, n_sel * SLC_BLOCK, HEAD_DIM)
        kpos = (sel[..., None] * SLC_BLOCK + jnp.arange(SLC_BLOCK)).reshape(b, KV_GROUPS, Q_BLOCK, n_sel * SLC_BLOCK)
        s_s = jnp.einsum("bqgrd,bgqkd->bgrqk", qb, ks) * scale
        p_s = masked_softmax(s_s, (kpos <= t[None, None, :, None])[:, :, None])
        o_s = jnp.einsum("bgrqk,bgqkd->bqgrd", p_s, vs)

        kw = lax.dynamic_slice_in_dim(k_win_pad, s0, Q_BLOCK + WINDOW, axis=1)
        vw = lax.dynamic_slice_in_dim(v_win_pad, s0, Q_BLOCK + WINDOW, axis=1)
        wpos = s0 - WINDOW + jnp.arange(Q_BLOCK + WINDOW)
        mask_w = (wpos[None, :] <= t[:, None]) & (wpos[None, :] > t[:, None] - WINDOW) & (wpos[None, :] >= 0)
        s_w = jnp.einsum("bqgrd,bkgd->bgrqk", qb, kw) * scale
        p_w = masked_softmax(s_w, mask_w)
        o_w = jnp.einsum("bgrqk,bkgd->bqgrd", p_w, vw)

        o = gb[..., 0:1] * o_c + gb[..., 1:2] * o_s + gb[..., 2:3] * o_w
        return o.reshape(b, Q_BLOCK, ATT_WIDTH).astype(q.dtype)

    out = lax.map(block, jnp.arange(s // Q_BLOCK))
    return out.transpose(1, 0, 2, 3).reshape(b, s, ATT_WIDTH)


def setup_inputs(seed: int = 0) -> dict:
    key = jax.random.key(seed)
    ks = jax.random.split(key, 32)

    def w(k, shape, fan_in):
        return jax.random.normal(k, shape, jnp.float32) * fan_in ** -0.5

    def gain(k, n):
        return 1.0 + 0.05 * jax.random.normal(k, (n,), jnp.float32)

    offs = jax.random.randint(ks[2], (BATCH, 1), 0, 2048, dtype=jnp.int32)
    return {
        "x": jax.random.normal(ks[0], (BATCH, SEQ, D_MODEL), jnp.float32),
        "mem": jax.random.normal(ks[1], (BATCH, MEM_LEN, D_MODEL), jnp.float32),
        "positions": jnp.arange(SEQ, dtype=jnp.int32)[None, :] + offs,
        "norm_mix_pre": gain(ks[3], D_MODEL),
        "w_in": w(ks[4], (D_MODEL, W_IN_COLS), D_MODEL),
        "conv_w": w(ks[5], (CONV_K, CONV_WIDTH), CONV_K),
        "pe_kc": 0.1 * jax.random.normal(ks[6], (CMP_BLOCK, HEAD_DIM), jnp.float32),
        "w1_kc": w(ks[7], (CMP_BLOCK * HEAD_DIM, CMP_HIDDEN), CMP_BLOCK * HEAD_DIM),
        "w2_kc": w(ks[8], (CMP_HIDDEN, HEAD_DIM), CMP_HIDDEN),
        "pe_vc": 0.1 * jax.random.normal(ks[9], (CMP_BLOCK, HEAD_DIM), jnp.float32),
        "w1_vc": w(ks[10], (CMP_BLOCK * HEAD_DIM, CMP_HIDDEN), CMP_BLOCK * HEAD_DIM),
        "w2_vc": w(ks[11], (CMP_HIDDEN, HEAD_DIM), CMP_HIDDEN),
        "norm_conv_out": gain(ks[12], CONV_WIDTH),
        "norm_attn_out": gain(ks[13], ATT_WIDTH),
        "w_out": w(ks[14], (MIX_WIDTH, D_MODEL), MIX_WIDTH),
        "norm_mix_post": gain(ks[15], D_MODEL),
        "norm_x_pre": gain(ks[16], D_MODEL),
        "norm_mem": gain(ks[17], D_MODEL),
        "w_q_x": w(ks[18], (D_MODEL, D_MODEL), D_MODEL),
        "w_kv_x": w(ks[19], (D_MODEL, 2 * D_MODEL), D_MODEL),
        "w_o_x": w(ks[20], (D_MODEL, D_MODEL), D_MODEL),
        "norm_x_post": gain(ks[21], D_MODEL),
        "norm_ffn_pre": gain(ks[22], D_MODEL),
        "w_gate_up": w(ks[23], (D_MODEL, 2 * D_FF), D_MODEL),
        "w_down": w(ks[24], (D_FF, D_MODEL), D_FF),
        "norm_ffn_post": gain(ks[25], D_MODEL),
    }


def reference(x, mem, positions, norm_mix_pre, w_in, conv_w, pe_kc, w1_kc, w2_kc, pe_vc, w1_vc, w2_vc,
              norm_conv_out, norm_attn_out, w_out, norm_mix_post, norm_x_pre, norm_mem, w_q_x, w_kv_x,
              w_o_x, norm_x_post, norm_ffn_pre, w_gate_up, w_down, norm_ffn_post):
    b, s, _ = x.shape
    h = x
    for _layer in range(DEPTH):
        u = rmsnorm(h, norm_mix_pre)
        proj = u @ w_in
        cuts = np.cumsum([CONV_WIDTH, CONV_WIDTH, CONV_WIDTH, ATT_WIDTH] + [KV_WIDTH] * 6)
        bg, cg, xin, q, kc, vc, ksl, vsl, kw, vw, gl = jnp.split(proj, cuts, axis=-1)
        kv_shape = (b, s, KV_GROUPS, HEAD_DIM)
        y_conv = short_conv_mixer(bg, cg, xin, conv_w)
        y_att = nsa_mixer(q.reshape(b, s, ATT_HEADS, HEAD_DIM), kc.reshape(kv_shape), vc.reshape(kv_shape),
                          ksl.reshape(kv_shape), vsl.reshape(kv_shape), kw.reshape(kv_shape), vw.reshape(kv_shape),
                          jax.nn.sigmoid(gl).reshape(b, s, ATT_HEADS, 3), positions,
                          pe_kc, w1_kc, w2_kc, pe_vc, w1_vc, w2_vc)
        mixed = jnp.concatenate([rmsnorm(y_conv, norm_conv_out), rmsnorm(y_att, norm_attn_out)], axis=-1)
        h = h + rmsnorm(mixed @ w_out, norm_mix_post)

        hn = rmsnorm(h, norm_x_pre)
        mn = rmsnorm(mem, norm_mem)
        qx = (hn @ w_q_x).reshape(b, s, X_HEADS, X_HEAD_DIM)
        kx, vx = jnp.split(mn @ w_kv_x, 2, axis=-1)
        kx = kx.reshape(b, MEM_LEN, X_HEADS, X_HEAD_DIM)
        vx = vx.reshape(b, MEM_LEN, X_HEADS, X_HEAD_DIM)
        sx = jnp.einsum("bshd,bmhd->bhsm", qx, kx).astype(jnp.float32) * X_HEAD_DIM ** -0.5
        px = jax.nn.softmax(sx, axis=-1)
        ox = jnp.einsum("bhsm,bmhd->bshd", px, vx).astype(h.dtype).reshape(b, s, D_MODEL)
        h = h + rmsnorm(ox @ w_o_x, norm_x_post)

        hn = rmsnorm(h, norm_ffn_pre)
        g, up = jnp.split(hn @ w_gate_up, 2, axis=-1)
        h = h + rmsnorm((jax.nn.silu(g) * up) @ w_down, norm_ffn_post)
    return h
```

```python
import numpy as np
from contextlib import ExitStack
import concourse.bass as bass
import concourse.mybir as mybir
from concourse.bass_utils import run_bass_kernel_spmd

F32 = mybir.dt.float32
BF16 = mybir.dt.bfloat16
I32 = mybir.dt.int32
AF = mybir.ActivationFunctionType
ALU = mybir.AluOpType
AX = mybir.AxisListType

D = 1024
DFF = 2816
MEM = 256
CH = 512
EPS = 1e-6
NEG = 30000.0
PI = float(np.pi)
C1 = 6.28125
C2 = float(2 * np.pi - 6.28125)


_LIMIT = None
_MARKS = []


class Buf:
    def __init__(self, t):
        self.t = t
        self.w = []
        self.r = {}

    def __getitem__(self, k):
        return self.t[k]


class Sch:
    def __init__(self, nc, es, n_dma=32):
        self.nc = nc
        self.E = {"pe": nc.tensor, "act": nc.scalar, "dve": nc.vector, "pool": nc.gpsimd, "sp": nc.sync}
        self.sem = {e: es.enter_context(nc.semaphore("s_" + e)) for e in self.E}
        self.cnt = {e: 0 for e in self.E}
        self.seen = {e: {} for e in self.E}
        self.dsem = [es.enter_context(nc.semaphore("d%d" % i)) for i in range(n_dma)]
        self.duse = [0] * n_dma
        self.qpool = {"pool": list(range(0, n_dma // 2)), "sp": list(range(n_dma // 2, n_dma))}
        self.qnext = {"pool": 0, "sp": 0}

    def _wait(self, eng, tok):
        kind, k, n = tok
        key = (kind, k)
        if self.seen[eng].get(key, 0) >= n:
            return
        if kind == "e":
            self.E[eng].wait_ge(self.sem[k], n)
        else:
            self.E[eng].wait_ge(self.dsem[k], 16 * n)
        self.seen[eng][key] = n

    def _deps(self, eng, reads, writes):
        isdma = eng in ("pool", "sp")
        for b in reads:
            for tok in b.w:
                self._wait(eng, tok)
        for b in writes:
            for tok in b.w:
                if not (isdma and tok[0] == "d"):
                    self._wait(eng, tok)
            for tok in b.r.values():
                self._wait(eng, tok)

    def _mark(self, tok, reads, writes):
        for b in reads:
            b.r[(tok[0], tok[1])] = tok
        for b in writes:
            if tok[0] == "d" and b.w and all(t[0] == "d" for t in b.w) and not b.r:
                b.w = b.w + [tok]
            else:
                b.w = [tok]
            b.r = {}

    def op(self, eng, fn, reads=(), writes=()):
        self.nops = getattr(self, "nops", 0) + 1
        if _LIMIT is not None and self.nops > _LIMIT:
            return
        self._deps(eng, reads, writes)
        inst = fn(self.E[eng])
        self.cnt[eng] += 1
        inst.then_inc(self.sem[eng], 1)
        self._mark(("e", eng, self.cnt[eng]), reads, writes)

    def dma(self, q, out, in_, reads=(), writes=(), force=False):
        self.nops = getattr(self, "nops", 0) + 1
        if _LIMIT is not None and self.nops > _LIMIT and not force:
            return
        pl = self.qpool[q]
        k = pl[self.qnext[q]]
        self.qnext[q] = (self.qnext[q] + 1) % len(pl)
        if self.duse[k] > 0:
            self._wait(q, ("d", k, self.duse[k]))
        self._deps(q, reads, writes)
        self.E[q].dma_start(out=out, in_=in_).then_inc(self.dsem[k], 16)
        self.duse[k] += 1
        self._mark(("d", k, self.duse[k]), reads, writes)

    def barrier(self, engs=("pe", "act", "dve"), dma=False):
        for e in tuple(engs) + (("pool", "sp") if dma else ()):
            for e2 in engs:
                if e2 != e and self.cnt[e2] > 0:
                    self._wait(e, ("e", e2, self.cnt[e2]))

    def final_wait(self, eng):
        for k in range(len(self.dsem)):
            if self.duse[k] > 0:
                self._wait(eng, ("d", k, self.duse[k]))


def build_nc(S):
    NCH = S // CH
    NT = S // 128
    NSLC = S // 64
    NCMP = (S - 32) // 16 + 1
    NCT = (NCMP + 127) // 128
    NCP = NCT * 128
    nc = bass.Bass("TRN2", target_bir_lowering=False)

    def din(name, shape, dt=F32):
        return nc.dram_tensor(name, shape, dt, kind="ExternalInput").ap()

    x_d = din("x", [S, D]); mem_d = din("mem", [MEM, D]); pos_d = din("pos", [1, S], I32)
    posc_d = din("posc", [1, NCP], I32)
    wk_d = din("wk", [D, 1280]); wq_d = din("wq", [D, 2560]); wg_d = din("wg", [D, 24])
    w1k_d = din("w1k", [2048, 256]); w1v_d = din("w1v", [2048, 256])
    w2k_d = din("w2k", [256, 256]); w2v_d = din("w2v", [256, 64])
    pek_d = din("pek", [128, 16]); pev_d = din("pev", [128, 16])
    cw_d = din("cw", [128, 12])
    gcols_d = din("gcols", [128, 40])
    gpost_d = din("gpost", [3, D])
    wout_d = din("w_out", [D, D]); wqx_d = din("w_q_x", [D, D]); wkvx_d = din("w_kv_x", [D, 2 * D])
    wox_d = din("w_o_x", [D, D]); wgu_d = din("w_gate_up", [D, 2 * DFF]); wd_d = din("w_down", [DFF, D])
    ident_d = din("ident", [128, 128]); ewide_d = din("ewide", [64, S]); tri_d = din("tri", [128, 256])
    cmpb_d = din("cmpb", [128, 4 * CH]); pat_d = din("pat", [128, 256]); maug_d = din("maug", [128, NCT * 64])
    cols_d = din("cols", [128, 4])
    out_d = nc.dram_tensor("out", [S, D], F32, kind="ExternalOutput").ap()

    es = ExitStack()
    with es:
        sch = Sch(nc, es)
        uid = [0]

        def sb(shape, dt, stack=es, name=None):
            uid[0] += 1
            return Buf(stack.enter_context(nc.sbuf_tensor((name or "t") + str(uid[0]), shape, dt)))

        def psb(shape, dt):
            uid[0] += 1
            return Buf(es.enter_context(nc.psum_tensor("p" + str(uid[0]), shape, dt)))

        PSG = [psb([128, 512], F32) for _ in range(4)]
        PST = [psb([128, 1024], BF16) for _ in range(2)]
        PSO = [psb([128, 512], F32) for _ in range(2)]
        rot = {"g": 0, "t": 0, "o": 0, "s": 0, "u": 0}

        def nxt(kind, pool):
            rot[kind] = (rot[kind] + 1) % len(pool)
            return pool[rot[kind]]

        ident = sb([128, 128], BF16); ones = sb([128, 128], BF16)
        tri = sb([128, 256], BF16); cmpb = sb([128, 4 * CH], BF16)
        pat = sb([128, 256], F32)
        cols = sb([128, 4], F32); gcols = sb([128, 40], F32); cw = sb([128, 12], F32)
        gpost = sb([128, 3, D], F32)
        WG = sb([128, 8, 24], BF16)
        KE = [sb([128, S], BF16), sb([128, S], BF16)]
        KW = sb([128, S], BF16)
        VV = sb([128, NT, 4, 65], BF16)
        KCMP = sb([128, NCP], BF16); CM = sb([128, NCT, 2, 128], BF16)
        KXT = sb([128, 8, MEM], BF16); VX = sb([128, 2, 4, 257], BF16)
        H = sb([128, 4, D], F32)
        U = [sb([128, D], BF16) for _ in range(2)]
        UT = sb([128, 8, CH], BF16)
        MT = sb([128, 8, CH], BF16)
        QS = sb([128, 8, CH], BF16)
        PT = [sb([128, CH], BF16) for _ in range(4)]
        STR = [sb([128, 4096], BF16) for _ in range(3)]
        st1 = sb([128, 8], F32)
        POSI = sb([128, CH], I32)

        sch.dma("pool", KE[0][64:128, :], ewide_d, writes=[KE[0]])
        sch.dma("pool", KE[1][0:64, :], ewide_d, writes=[KE[1]])
        for g_ in range(2):
            sch.dma("pool", CM[:, :, g_, 64:128], maug_d.rearrange("p (j s) -> p j s", j=NCT), writes=[CM])
        for (dst, src) in ((ident, ident_d), (tri, tri_d), (cmpb, cmpb_d)):
            sch.dma("pool", dst[:], src, writes=[dst])
        for (dst, src) in ((pat, pat_d), (cols, cols_d), (gcols, gcols_d), (cw, cw_d)):
            sch.dma("sp", dst[:], src, writes=[dst])
        for i in range(3):
            sch.dma("sp", gpost[:, i, :], gpost_d[i:i + 1, :].partition_broadcast(128), writes=[gpost])
        sch.dma("pool", WG[:], wg_d.rearrange("(kc p) n -> p kc n", p=128), writes=[WG])
        sch.op("dve", lambda e: e.memset(ones[:], 1.0), writes=[ones])
        sch.op("dve", lambda e: e.memset(VV[:], 1.0), writes=[VV])
        sch.op("dve", lambda e: e.memset(VX[:], 1.0), writes=[VX])
        INV = cols[:, 0:1]; SIGN = cols[:, 1:2]; EPSC = cols[:, 2:3]

        def stream(parts):
            b = nxt("s", STR)
            for vf, src in parts:
                sch.dma("pool", vf(b), src, writes=[b])
            return b

        def v3(b, a, c):
            return b[:, 0:a * c].rearrange("p (a c) -> p a c", a=a)

        def rms_rstd(src_ap, src_bufs, n, col, junk=None):
            junk = junk if junk is not None else nxt("u", U)
            sch.op("dve", lambda e: e.memset(st1[:, col:col + 1], 0.0), writes=[st1])
            sch.op("act", lambda e: e.activation(out=junk[:, 0:src_ap.shape[-1]], in_=src_ap, func=AF.Square,
                                                 accum_out=st1[:, col:col + 1]),
                   reads=src_bufs, writes=[junk, st1])

        def fin_rstd(col, n):
            sch.op("act", lambda e: e.activation(out=st1[:, col:col + 1], in_=st1[:, col:col + 1], func=AF.Sqrt,
                                                 bias=EPSC, scale=1.0 / n), reads=[st1, cols], writes=[st1])
            sch.op("dve", lambda e: e.reciprocal(st1[:, col:col + 1], st1[:, col:col + 1]), reads=[st1], writes=[st1])

        def norm_T(src_ap, src_bufs, gofs, dstT, tt, nk=8):
            ub = nxt("u", U)
            rms_rstd(src_ap, src_bufs, nk * 128, 0, junk=ub)
            fin_rstd(0, nk * 128)
            sch.op("dve", lambda e: e.tensor_scalar(ub[:, 0:nk * 128], src_ap, st1[:, 0:1], None, ALU.mult),
                   reads=src_bufs + [st1], writes=[ub])
            transp(ub, nk, gofs, dstT, tt)

        def transp(ub, nk, gofs, dstT, tt, kofs=0):
            pt = nxt("t", PST)

            def f(e):
                for k in range(nk):
                    ins = e.transpose(pt[:, k * 128:(k + 1) * 128], ub[:, k * 128:(k + 1) * 128], ident[:])
                return ins
            sch.op("pe", f, reads=[ub, ident], writes=[pt])
            dst = dstT[:, kofs:kofs + nk, tt * 128:(tt + 1) * 128]
            src = pt[:, 0:nk * 128].rearrange("p (k c) -> p k c", k=nk)
            if gofs is None:
                sch.op("act", lambda e: e.activation(out=dst, in_=src, func=AF.Copy), reads=[pt], writes=[dstT])
            else:
                g = gcols[:, gofs:gofs + nk].unsqueeze(2).broadcast_to([128, nk, 128])
                sch.op("dve", lambda e: e.tensor_tensor(dst, src, g, ALU.mult), reads=[pt, gcols], writes=[dstT])

        def proj_fm(W_ap_fn, wbufs, rhsT, ncol=CH, nk=8, M=128):
            ps = nxt("g", PSG)

            def f(e):
                for k in range(nk):
                    ins = e.matmul(ps[0:M, 0:ncol], W_ap_fn(k), rhsT[:, k, 0:ncol], start=(k == 0), stop=(k == nk - 1))
                return ins
            sch.op("pe", f, reads=wbufs + [rhsT], writes=[ps])
            return ps

        def tables(stack, posi, n, COS, SINS):
            ang = sb([128, n], F32, stack); ki = sb([128, n], I32, stack); kf = sb([128, n], F32, stack)
            r = sb([128, n], F32, stack)
            sch.op("dve", lambda e: e.tensor_copy(ang[:], posi[:]), reads=[posi], writes=[ang])
            sch.op("dve", lambda e: e.tensor_scalar(ang[:], ang[:], INV, None, ALU.mult), reads=[ang, cols], writes=[ang])
            for shift, dst, sc in ((0.0, SINS, SIGN), (PI / 2, COS, None)):
                sch.op("dve", lambda e, s=shift: e.tensor_scalar(ki[:], ang[:], 1.0 / (2 * PI), s / (2 * PI), ALU.mult, ALU.add),
                       reads=[ang], writes=[ki])
                sch.op("dve", lambda e: e.tensor_copy(kf[:], ki[:]), reads=[ki], writes=[kf])
                sch.op("dve", lambda e: e.scalar_tensor_tensor(out=r[:], in0=kf[:], scalar=-C1, in1=ang[:], op0=ALU.mult, op1=ALU.add),
                       reads=[kf, ang], writes=[r])
                sch.op("dve", lambda e: e.scalar_tensor_tensor(out=r[:], in0=kf[:], scalar=-C2, in1=r[:], op0=ALU.mult, op1=ALU.add),
                       reads=[kf, r], writes=[r])
                sch.op("dve", lambda e, s=shift: e.tensor_scalar(r[:], r[:], s, 3.14159, ALU.add, ALU.min), reads=[r], writes=[r])
                sch.op("dve", lambda e: e.tensor_scalar(r[:], r[:], -3.14159, None, ALU.max), reads=[r], writes=[r])
                if sc is None:
                    sch.op("act", lambda e, d=dst: e.activation(out=d[:], in_=r[:], func=AF.Sin), reads=[r], writes=[dst])
                else:
                    sch.op("act", lambda e, d=dst: e.activation(out=d[:], in_=r[:], func=AF.Sin, scale=SIGN),
                           reads=[r, cols], writes=[dst])

        def rope(stack_tmp, A, Asw, COS, SINS, dst_ap, dst_buf, n, rows=slice(0, 128), cs=None):
            t1, t2 = stack_tmp
            cosap = COS[rows, 0:n] if cs is None else cs(COS)
            sinap = SINS[rows, 0:n] if cs is None else cs(SINS)
            sch.op("dve", lambda e: e.tensor_tensor(t1[rows, 0:n], A[rows, 0:n], cosap, ALU.mult), reads=[A, COS], writes=[t1])
            sch.op("dve", lambda e: e.tensor_tensor(t2[rows, 0:n], Asw[rows, 0:n], sinap, ALU.mult), reads=[Asw, SINS], writes=[t2])
            sch.op("dve", lambda e: e.tensor_tensor(dst_ap, t1[rows, 0:n], t2[rows, 0:n], ALU.add), reads=[t1, t2], writes=[dst_buf])

        def mark(name):
            _MARKS.append((name, getattr(sch, "nops", 0), dict(sch.cnt)))

        def load_x_chunk(c):
            for tt in range(4):
                sch.dma("sp", H[:, tt, :], x_d[c * CH + tt * 128: c * CH + (tt + 1) * 128, :], writes=[H])

        mark("consts_done")
        with ExitStack() as ps0:
            mt_ = sb([128, 2, D], F32, ps0)
            mnT = sb([128, 8, MEM], BF16, ps0)
            for t in range(2):
                sch.dma("sp", mt_[:, t, :], mem_d[t * 128:(t + 1) * 128, :], writes=[mt_])
            for t in range(2):
                norm_T(mt_[:, t, :], [mt_], 24, mnT, t)
            for u in range(2):
                wb = stream([(lambda b: v3(b, 8, 512), wkvx_d[:, u * 512:(u + 1) * 512].rearrange("(kc p) n -> p kc n", p=128))])
                w3 = v3(wb, 8, 512)
                for j in range(4):
                    ps = proj_fm(lambda k, j=j, w3=w3: w3[:, k, j * 128:(j + 1) * 128], [wb], mnT, ncol=MEM)
                    sch.op("act", lambda e, ps=ps, ct=u * 4 + j: e.activation(out=KXT[:, ct, :], in_=ps[:, 0:MEM], func=AF.Copy),
                           reads=[ps], writes=[KXT])
            for u in range(2):
                wb = stream([(lambda b: v3(b, 8, 512), wkvx_d[:, D + u * 512: D + (u + 1) * 512].rearrange("(kc p) n -> p kc n", p=128))])
                w3 = v3(wb, 8, 512)
                for t in range(2):
                    ps = nxt("g", PSG)

                    def f(e, ps=ps, w3=w3, t=t):
                        for k in range(8):
                            ins = e.matmul(ps[:, :], mnT[:, k, t * 128:(t + 1) * 128], w3[:, k, :], start=(k == 0), stop=(k == 7))
                        return ins
                    sch.op("pe", f, reads=[wb, mnT], writes=[ps])
                    sch.op("act", lambda e, ps=ps, t=t, u=u: e.activation(
                        out=VX[:, t, 2 * u:2 * u + 2, 0:256], in_=ps[:, :].rearrange("p (h c) -> p h c", h=2), func=AF.Copy),
                        reads=[ps], writes=[VX])
            sch.barrier(dma=True)

        mark("phase0_done")
        with ExitStack() as pk:
            WK = sb([128, 8, 1280], BF16, pk)
            W1 = [sb([128, 16, 256], BF16, pk) for _ in range(2)]
            W2K = sb([128, 2, 256], BF16, pk); W2V = sb([128, 2, 64], BF16, pk)
            PE_ = sb([128, 32], BF16, pk)
            HB = sb([128, 8], F32, pk)
            HTA = [sb([128, 2, 2, NCP], BF16, pk) for _ in range(2)]
            KCc = [[sb([128, 528], BF16, pk)] for _ in range(4)]
            COS = sb([128, CH], F32, pk); SINS = sb([128, CH], F32, pk)
            posi = POSI
            pci = sb([128, NCP], I32, pk)
            t1 = sb([128, CH], F32, pk); t2 = sb([128, CH], F32, pk)
            sg = sb([128, 32], F32, pk)
            for k in range(8):
                sch.dma("pool", WK[:, k, :], wk_d[k * 128:(k + 1) * 128, :], writes=[WK])
            for i, wd_ in enumerate((w1k_d, w1v_d)):
                for hh in range(2):
                    sch.dma("pool", W1[i][:, hh * 8:(hh + 1) * 8, :],
                            wd_[hh * 1024:(hh + 1) * 1024, :].rearrange("(m p) n -> p m n", p=128), writes=[W1[i]])
            sch.dma("pool", W2K[:], w2k_d.rearrange("(hc p) n -> p hc n", p=128), writes=[W2K])
            sch.dma("pool", W2V[:], w2v_d.rearrange("(hc p) n -> p hc n", p=128), writes=[W2V])
            sch.dma("pool", PE_[:, 0:16], pek_d, writes=[PE_])
            sch.dma("pool", PE_[:, 16:32], pev_d, writes=[PE_])
            for bl in KCc:
                for b in bl:
                    sch.op("dve", lambda e, b=b: e.memset(b[:], 0.0), writes=[b])
            for i in range(2):
                sch.op("dve", lambda e, i=i: e.memset(HTA[i][:], 0.0), writes=[HTA[i]])
            for kv in range(2):
                for hc in range(2):
                    ps = nxt("g", PSG)

                    def f(e, ps=ps, kv=kv, hc=hc):
                        for m in range(16):
                            ins = e.matmul(ps[:, 0:1], W1[kv][:, m, hc * 128:(hc + 1) * 128], PE_[:, kv * 16 + m: kv * 16 + m + 1],
                                           start=(m == 0), stop=(m == 15))
                        return ins
                    sch.op("pe", f, reads=[W1[kv], PE_], writes=[ps])
                    sch.op("act", lambda e, ps=ps, c_=kv * 2 + hc: e.activation(out=HB[:, c_:c_ + 1], in_=ps[:, 0:1], func=AF.Copy),
                           reads=[ps], writes=[HB])

            mark("pk_setup_done")
            for c in range(NCH):
                mark("pk_chunk%d" % c)
                load_x_chunk(c)
                sch.dma("sp", posi[:], pos_d[:, c * CH:(c + 1) * CH].partition_broadcast(128), writes=[posi])
                for tt in range(4):
                    norm_T(H[:, tt, :], [H], 0, UT, tt)
                with ExitStack() as tk:
                    tables(tk, posi, CH, COS, SINS)
                    sch.barrier()
                for kind in (0, 2):
                    A = proj_fm(lambda k, ct=kind: WK[:, k, ct * 128:(ct + 1) * 128], [WK], UT)
                    Asw = proj_fm(lambda k, ct=kind + 1: WK[:, k, ct * 128:(ct + 1) * 128], [WK], UT)
                    if kind == 2:
                        rope((t1, t2), A, Asw, COS, SINS, KW[:, c * CH:(c + 1) * CH], KW, CH)
                    else:
                        for g in range(2):
                            rws = slice(64 * g, 64 * g + 64)
                            rope((t1, t2), A, Asw, COS, SINS, KE[g][rws, c * CH:(c + 1) * CH], KE[g], CH, rows=rws)
                pp = c % 2
                for kv in range(2):
                    for g in range(2):
                        cur = KCc[kv * 2 + g][0]; prev = cur
                        ct = 4 + kv * 2 + g
                        ps = proj_fm(lambda k, ct=ct: WK[:, k, ct * 128:(ct + 1) * 128], [WK], UT)
                        if c > 0:
                            sch.op("dve", lambda e, cur=cur, prev=prev: e.tensor_copy(cur[:, 0:16], prev[:, 512:528]),
                                   reads=[prev], writes=[cur])
                        sch.op("act", lambda e, cur=cur, ps=ps: e.activation(out=cur[0:64, 16:528], in_=ps[0:64, :], func=AF.Copy),
                               reads=[ps], writes=[cur])
                        sch.op("act", lambda e, cur=cur, ps=ps: e.activation(out=cur[64:128, 15:527], in_=ps[64:128, :], func=AF.Copy),
                               reads=[ps], writes=[cur])
                        i0 = 32 * c - 1
                        lo = 1 if c == 0 else 0
                        for hc in range(2):
                            ph = nxt("g", PSG)

                            def f(e, ph=ph, kv=kv, hc=hc, cur=cur):
                                for m in range(16):
                                    rhs = cur[:, 2 * m: 2 * m + 16 * 31 + 1: 16]
                                    ins = e.matmul(ph[:, 0:32], W1[kv][:, m, hc * 128:(hc + 1) * 128], rhs, start=(m == 0), stop=(m == 15))
                                return ins
                            sch.op("pe", f, reads=[W1[kv], cur], writes=[ph])
                            bc = HB[:, kv * 2 + hc: kv * 2 + hc + 1]
                            sch.op("act", lambda e, ph=ph, bc=bc: e.activation(out=sg[:, :], in_=ph[:, 0:32], func=AF.Sigmoid, bias=bc),
                                   reads=[ph, HB], writes=[sg])
                            sch.op("dve", lambda e, ph=ph, bc=bc, kv=kv, hc=hc, g=g, lo=lo, i0=i0: e.scalar_tensor_tensor(
                                out=HTA[kv][:, hc, g, i0 + lo: i0 + 32], in0=ph[:, lo:32], scalar=bc, in1=sg[:, lo:32],
                                op0=ALU.add, op1=ALU.mult), reads=[ph, sg, HB], writes=[HTA[kv]])
                for tt in range(4):
                    ps = nxt("g", PSG)

                    def f(e, ps=ps, tt=tt):
                        for k in range(8):
                            ins = e.matmul(ps[:, 0:256], UT[:, k, tt * 128:(tt + 1) * 128], WK[:, k, 1024:1280], start=(k == 0), stop=(k == 7))
                        return ins
                    sch.op("pe", f, reads=[UT, WK], writes=[ps])
                    sch.op("act", lambda e, ps=ps, ti=c * 4 + tt: e.activation(
                        out=VV[:, ti, :, 0:64], in_=ps[:, 0:256].rearrange("p (a d) -> p a d", a=4), func=AF.Copy),
                        reads=[ps], writes=[VV])
            mark("pk_chunks_done")
            with ExitStack() as tk:
                COSc = sb([128, NCP], F32, tk); SINc = sb([128, NCP], F32, tk)
                sch.dma("sp", pci[:], posc_d.partition_broadcast(128), writes=[pci])
                tables(tk, pci, NCP, COSc, SINc)
                for g in range(2):
                    rows = slice(64 * g, 64 * g + 64)
                    for j in range(NCT):
                        pa = nxt("g", PSG); pb = nxt("g", PSG)
                        for (pp_, off) in ((pa, 0), (pb, 128)):
                            def f(e, pp_=pp_, off=off, g=g, j=j):
                                for hc in range(2):
                                    ins = e.matmul(pp_[:, 0:128], W2K[:, hc, off:off + 128], HTA[0][:, hc, g, j * 128:(j + 1) * 128],
                                                   start=(hc == 0), stop=(hc == 1))
                                return ins
                            sch.op("pe", f, reads=[W2K, HTA[0]], writes=[pp_])
                        rope((t1, t2), pa, pb, COSc, SINc, KCMP[rows, j * 128:(j + 1) * 128], KCMP, 128, rows=rows,
                             cs=lambda T, j=j, rows=rows: T[rows, j * 128:(j + 1) * 128])
                        pv = nxt("g", PSG)

                        def f(e, pv=pv, g=g, j=j):
                            for hc in range(2):
                                ins = e.matmul(pv[:, 0:64], HTA[1][:, hc, g, j * 128:(j + 1) * 128], W2V[:, hc, :], start=(hc == 0), stop=(hc == 1))
                            return ins
                        sch.op("pe", f, reads=[W2V, HTA[1]], writes=[pv])
                        sch.op("act", lambda e, pv=pv, g=g, j=j: e.activation(out=CM[:, j, g, 0:64], in_=pv[:, 0:64], func=AF.Copy),
                               reads=[pv], writes=[CM])
                sch.barrier()
            sch.barrier(dma=True)
        STR.extend([sb([128, 4096], BF16) for _ in range(2)])

        def wstream8(src_ap_cols):
            wb = stream([(lambda b, h=h: v3(b, 8, 512)[:, 4 * h:4 * h + 4, :],
                          src_ap_cols[h * 512:(h + 1) * 512, :].rearrange("(kc p) n -> p kc n", p=128)) for h in range(2)])
            return wb, v3(wb, 8, 512)

        def out_proj_add(srcT, w_d, gi):
            halves = [wstream8(w_d[:, hf * 512:(hf + 1) * 512]) for hf in range(2)]
            for tt in range(4):
                pss = []
                for hf in range(2):
                    wb, w3 = halves[hf]
                    ps = nxt("g", PSG)

                    def f(e, ps=ps, w3=w3, tt=tt):
                        for k in range(8):
                            ins = e.matmul(ps[:, :], srcT[:, k, tt * 128:(tt + 1) * 128], w3[:, k, :], start=(k == 0), stop=(k == 7))
                        return ins
                    sch.op("pe", f, reads=[wb, srcT], writes=[ps])
                    rms_rstd(ps[:, :], [ps], D, 2 + hf)
                    pss.append(ps)
                sch.op("dve", lambda e: e.tensor_tensor(st1[:, 2:3], st1[:, 2:3], st1[:, 3:4], ALU.add), reads=[st1], writes=[st1])
                fin_rstd(2, D)
                for hf in range(2):
                    sch.op("dve", lambda e, ps=pss[hf], hf=hf: e.scalar_tensor_tensor(
                        out=ftmp[:, :], in0=ps[:, :], scalar=st1[:, 2:3], in1=gpost[:, gi, hf * 512:(hf + 1) * 512],
                        op0=ALU.mult, op1=ALU.mult), reads=[pss[hf], st1, gpost], writes=[ftmp])
                    sch.op("dve", lambda e, hf=hf, tt=tt: e.tensor_tensor(
                        H[:, tt, hf * 512:(hf + 1) * 512], H[:, tt, hf * 512:(hf + 1) * 512], ftmp[:, :], ALU.add),
                        reads=[ftmp, H], writes=[H])

        ftmp = sb([128, CH], F32)
        G = sb([128, 4, 24], F32)
        ZHIST = sb([128, 4, 2], F32)
        sch.op("dve", lambda e: e.memset(ZHIST[:], 0.0), writes=[ZHIST])

        for c in range(NCH):
            mark("pq_chunk%d" % c)
            load_x_chunk(c)
            with ExitStack() as sa:
                COS = sb([128, CH], F32, sa); SINS = sb([128, CH], F32, sa); posi = POSI
                t1 = sb([128, CH], F32, sa); t2 = sb([128, CH], F32, sa)
                sch.dma("sp", posi[:], pos_d[:, c * CH:(c + 1) * CH].partition_broadcast(128), writes=[posi])
                for tt in range(4):
                    norm_T(H[:, tt, :], [H], 0, UT, tt)
                with ExitStack() as tk:
                    tables(tk, posi, CH, COS, SINS)
                    sch.barrier()
                for tt in range(4):
                    ps = nxt("g", PSG)

                    def f(e, ps=ps, tt=tt):
                        for k in range(8):
                            ins = e.matmul(ps[:, 0:24], UT[:, k, tt * 128:(tt + 1) * 128], WG[:, k, :], start=(k == 0), stop=(k == 7))
                        return ins
                    sch.op("pe", f, reads=[UT, WG], writes=[ps])
                    sch.op("act", lambda e, ps=ps, tt=tt: e.activation(out=G[:, tt, :], in_=ps[:, 0:24], func=AF.Sigmoid),
                           reads=[ps], writes=[G])
                wqs = [wstream8(wq_d[:, u * 512:(u + 1) * 512]) for u in range(2)]
                for p in range(4):
                    wbA, wA = wqs[0]; wbS, wS = wqs[1]
                    A = proj_fm(lambda k, p=p, wA=wA: wA[:, k, p * 128:(p + 1) * 128], [wbA], UT)
                    Asw = proj_fm(lambda k, p=p, wS=wS: wS[:, k, p * 128:(p + 1) * 128], [wbS], UT)
                    for g in range(2):
                        rws = slice(64 * g, 64 * g + 64)
                        rope((t1, t2), A, Asw, COS, SINS, QS[rws, p + 4 * g, :], QS, CH, rows=rws)
                sch.barrier()
            with ExitStack() as sa:
                Zc = sb([128, 4, 514], F32, sa); Y = sb([128, 4, CH], F32, sa); YSQ = sb([128, 4, CH], BF16, sa)
                xin = sb([128, CH], F32, sa); t1 = sb([128, CH], F32, sa); RB = sb([128, CH], F32, sa)
                wcs = [wstream8(wq_d[:, 1024 + u * 512: 1024 + (u + 1) * 512]) for u in range(3)]
                for ct in range(4):
                    pb = proj_fm(lambda k, ct=ct, w=wcs[0][1]: w[:, k, ct * 128:(ct + 1) * 128], [wcs[0][0]], UT)
                    pc = proj_fm(lambda k, ct=ct, w=wcs[1][1]: w[:, k, ct * 128:(ct + 1) * 128], [wcs[1][0]], UT)
                    px = proj_fm(lambda k, ct=ct, w=wcs[2][1]: w[:, k, ct * 128:(ct + 1) * 128], [wcs[2][0]], UT)
                    sch.op("act", lambda e, px=px: e.activation(out=xin[:, :], in_=px[:, :], func=AF.Copy), reads=[px], writes=[xin])
                    sch.op("dve", lambda e, ct=ct: e.tensor_copy(Zc[:, ct, 0:2], ZHIST[:, ct, :]), reads=[ZHIST], writes=[Zc])
                    sch.op("dve", lambda e, pc=pc, ct=ct: e.tensor_tensor(Zc[:, ct, 2:514], pc[:, :], xin[:, :], ALU.mult),
                           reads=[pc, xin], writes=[Zc])
                    sch.op("dve", lambda e, ct=ct: e.tensor_copy(ZHIST[:, ct, :], Zc[:, ct, 512:514]), reads=[Zc], writes=[ZHIST])
                    sch.op("dve", lambda e, ct=ct: e.tensor_scalar(t1[:, :], Zc[:, ct, 2:514], cw[:, ct * 3 + 2: ct * 3 + 3], None, ALU.mult),
                           reads=[Zc, cw], writes=[t1])
                    for kk in (1, 0):
                        sch.op("dve", lambda e, ct=ct, kk=kk: e.scalar_tensor_tensor(
                            out=t1[:, :], in0=Zc[:, ct, kk:kk + 512], scalar=cw[:, ct * 3 + kk: ct * 3 + kk + 1], in1=t1[:, :],
                            op0=ALU.mult, op1=ALU.add), reads=[Zc, cw, t1], writes=[t1])
                    sch.op("dve", lambda e, pb=pb, ct=ct: e.tensor_tensor(Y[:, ct, :], pb[:, :], t1[:, :], ALU.mult),
                           reads=[pb, t1], writes=[Y])
                    sch.op("act", lambda e, ct=ct: e.activation(out=YSQ[:, ct, :], in_=Y[:, ct, :], func=AF.Square), reads=[Y], writes=[YSQ])
                ps = nxt("g", PSG)

                def f(e, ps=ps):
                    for ct in range(4):
                        ins = e.matmul(ps[:, :], ones[:], YSQ[:, ct, :], start=(ct == 0), stop=(ct == 3))
                    return ins
                sch.op("pe", f, reads=[ones, YSQ], writes=[ps])
                sch.op("act", lambda e, ps=ps: e.activation(out=RB[:, :], in_=ps[:, :], func=AF.Sqrt, bias=EPSC, scale=1.0 / 512),
                       reads=[ps, cols], writes=[RB])
                sch.op("dve", lambda e: e.reciprocal(RB[:, :], RB[:, :]), reads=[RB], writes=[RB])
                for ct in range(4):
                    sch.op("dve", lambda e, ct=ct: e.scalar_tensor_tensor(
                        out=MT[:, ct, :], in0=Y[:, ct, :], scalar=gcols[:, 32 + ct:33 + ct], in1=RB[:, :], op0=ALU.mult, op1=ALU.mult),
                        reads=[Y, RB, gcols], writes=[MT])
                sch.barrier()

            mark("(b) c%d" % c)
            with ExitStack() as sa:
                YATT = sb([128, 4, 512], F32, sa)
                IMP = sb([128, 2, 4, 64], F32, sa)
                imp2 = sb([128, 64], F32, sa); wrk = sb([128, 64], F32, sa); mx = sb([128, 16], F32, sa)
                nbq = sb([128, 128], BF16, sa)
                sm = sb([128, 8], F32, sa)

                def finish(po, h, br, first, ow=65, with_imp=False):
                    g = h // 4
                    for tt in range(4):
                        o = po[:, tt * ow: tt * ow + 64]
                        if with_imp:
                            io = po[:, tt * ow + 64: tt * ow + 128]
                            sch.op("dve", lambda e, io=io: e.tensor_reduce(sm[:, 0:1], io, AX.X, ALU.add), reads=[po], writes=[sm])
                            sch.op("dve", lambda e: e.tensor_scalar(sm[:, 0:1], sm[:, 0:1], 1e-30, None, ALU.max), reads=[sm], writes=[sm])
                        else:
                            dn = po[:, tt * ow + 64: tt * ow + 65]
                            sch.op("dve", lambda e, dn=dn: e.tensor_scalar(sm[:, 0:1], dn, 1e-30, None, ALU.max), reads=[po], writes=[sm])
                        sch.op("dve", lambda e: e.reciprocal(sm[:, 0:1], sm[:, 0:1]), reads=[sm], writes=[sm])
                        sch.op("dve", lambda e, tt=tt: e.tensor_tensor(sm[:, 1:2], sm[:, 0:1], G[:, tt, 3 * h + br: 3 * h + br + 1], ALU.mult),
                               reads=[sm, G], writes=[sm])
                        ya = YATT[:, tt, 64 * h: 64 * h + 64]
                        if first:
                            sch.op("dve", lambda e, o=o, ya=ya: e.tensor_scalar(ya, o, sm[:, 1:2], None, ALU.mult),
                                   reads=[po, sm], writes=[YATT])
                        else:
                            sch.op("dve", lambda e, o=o, ya=ya: e.scalar_tensor_tensor(out=ya, in0=o, scalar=sm[:, 1:2], in1=ya,
                                                                                 op0=ALU.mult, op1=ALU.add),
                                   reads=[po, sm, YATT], writes=[YATT])
                        if with_imp:
                            dst = IMP[:, g, tt, :]
                            if h % 4 == 0:
                                sch.op("dve", lambda e, io=io, dst=dst: e.tensor_scalar(dst, io, sm[:, 0:1], None, ALU.mult),
                                       reads=[po, sm], writes=[IMP])
                            else:
                                sch.op("dve", lambda e, io=io, dst=dst: e.scalar_tensor_tensor(out=dst, in0=io, scalar=sm[:, 0:1], in1=dst,
                                                                                         op0=ALU.mult, op1=ALU.add),
                                       reads=[po, sm, IMP], writes=[IMP])

                def attend(h, tiles, ktile_ap, vtile_ap, vbufs, extra, po, full_k=False, ow=65):
                    g = h // 4
                    rows = slice(64 * g, 64 * g + 64)
                    for pz in [po]:
                        while any(pz in tg for (_, tg) in pend):
                            pend.pop(0)[0]()
                        sch.op("dve", lambda e, pz=pz: e.memset(pz[:, 0:4 * ow], 0.0), writes=[pz])
                    for (j, lo, hi) in tiles:
                        while len(pend) > LOOK:
                            pend.pop(0)[0]()
                        ps = nxt("g", PSG)
                        pt = PT[(rot_pt[0]) % 4]
                        rot_pt[0] += 1
                        cl, chh = lo * 128, hi * 128
                        if full_k:
                            mms = [(ktile_ap(j, rows), QS[:, h, cl:chh], (cl, chh))] + extra(j, lo, hi)
                        else:
                            mms = [(ktile_ap(j, rows), QS[rows, h, cl:chh], (cl, chh))] + extra(j, lo, hi)

                        def f(e, ps=ps, mms=mms):
                            n = len(mms)
                            for i, (l, r, (a, b)) in enumerate(mms):
                                ins = e.matmul(ps[:, a:b], l, r, start=(i == 0), stop=(i == n - 1))
                            return ins
                        sch.op("pe", f, reads=[KE[0], KE[1], KW, KCMP, QS, ident, tri, cmpb], writes=[ps])
                        sch.op("act", lambda e, ps=ps, pt=pt, cl=cl, chh=chh: e.activation(out=pt[:, cl:chh], in_=ps[:, cl:chh], func=AF.Exp,
                                                                                  scale=0.125), reads=[ps], writes=[pt])

                        def f2(e, pt=pt, j=j, lo=lo, hi=hi):
                            for tt in range(lo, hi):
                                ins = e.matmul(po[:, tt * ow: tt * ow + ow], pt[:, tt * 128:(tt + 1) * 128], vtile_ap(j, g),
                                               start=False, stop=False, skip_group_check=True)
                            return ins
                        pend.append((lambda f2=f2, pt=pt: sch.op("pe", f2, reads=[pt] + vbufs, writes=[po]), (po,)))

                def flush():
                    while pend:
                        pend.pop(0)[0]()

                pend = []
                LOOK = 3
                rot_pt = [0]
                jt_c = [j for j in range(NCT) if c - 4 * j >= 0]

                def first_last(j, qi):
                    return (j == jt_c[0], j == jt_c[-1])

                def extra_c(j, lo, hi):
                    dl = c - 4 * j
                    if dl <= 3:
                        return [(ident[:], cmpb[:, dl * CH:(dl + 1) * CH], (0, CH))]
                    return []
                for h in range(8):
                    po = nxt("o", PSO)
                    attend(h, [(j, 0, 4) for j in jt_c], lambda j, rows: KCMP[rows, j * 128:(j + 1) * 128],
                           lambda j, g: CM[:, j, g, :], [CM], extra_c, po, ow=128)
                    pend.append((lambda po=po, h=h: finish(po, h, 0, True, ow=128, with_imp=True), (po,)))
                flush()
                mark("topk c%d" % c)
                for g in range(2):
                    for tt in range(4):
                        qi = 4 * c + tt
                        a0 = 64 - 2 * qi
                        sch.op("dve", lambda e, g=g, tt=tt, a0=a0: e.tensor_tensor(imp2[:, :], IMP[:, g, tt, :], pat[:, a0:a0 + 64], ALU.mult),
                               reads=[IMP, pat], writes=[imp2])
                        sch.op("dve", lambda e, a0=a0: e.tensor_tensor(imp2[:, :], imp2[:, :], pat[:, 128 + a0:128 + a0 + 64], ALU.add),
                               reads=[imp2, pat], writes=[imp2])
                        sch.op("dve", lambda e: e.memset(imp2[:, 0:1], 1.0e4), reads=[imp2], writes=[imp2])
                        sch.op("dve", lambda e: e.max(mx[:, 0:8], imp2[:, :]), reads=[imp2], writes=[mx])
                        sch.op("dve", lambda e: e.match_replace(wrk[:, :], mx[:, 0:8], imp2[:, :], -1e30), reads=[mx, imp2], writes=[wrk])
                        sch.op("dve", lambda e: e.max(mx[:, 8:16], wrk[:, :]), reads=[wrk], writes=[mx])
                        sch.op("dve", lambda e: e.tensor_reduce(sm[:, 2:3], mx[:, 8:16], AX.X, ALU.min), reads=[mx], writes=[sm])
                        for hf in range(2):
                            sch.op("dve", lambda e, hf=hf: e.tensor_scalar(nbq[:, 64 * hf:64 * hf + 64], imp2[:, :], sm[:, 2:3], 1.0, ALU.is_ge, ALU.subtract),
                                   reads=[imp2, sm], writes=[nbq])
                        pt_ = nxt("t", PST)
                        sch.op("pe", lambda e, pt_=pt_: e.transpose(pt_[:, 0:128], nbq[:, :], ident[:]), reads=[nbq, ident], writes=[pt_])
                        mr = slice(64 * (1 - g), 64 * (1 - g) + 64)
                        dst4 = QS[mr, 4 * g:4 * g + 4, tt * 128:(tt + 1) * 128]
                        src4 = pt_[mr, 0:128].unsqueeze(1).broadcast_to([64, 4, 128])
                        sch.op("act", lambda e, dst4=dst4, src4=src4: e.activation(out=dst4, in_=src4, func=AF.Copy, scale=NEG),
                               reads=[pt_], writes=[QS])
                mark("br2 c%d" % c)
                def first_last(j, qi):
                    return (j == 0, j == qi)

                for h in range(8):
                    g = h // 4

                    def extra_s(j, lo, hi, g=g):
                        ex = []
                        if j >= 4 * c:
                            d0 = (j - 4 * c) * 128
                            ex.append((ident[:], tri[:, 0:128], (d0, d0 + 128)))
                        return ex
                    po = nxt("o", PSO)
                    attend(h, [(j, max(0, j - 4 * c), 4) for j in range(4 * c + 4)], lambda j, rows, g=g: KE[g][:, j * 128:(j + 1) * 128],
                           lambda j, g: VV[:, j, g, :], [VV], extra_s, po, full_k=True)
                    pend.append((lambda po=po, h=h: finish(po, h, 1, False), (po,)))
                flush()
                mark("br3 c%d" % c)
                def first_last(j, qi):
                    return (j == max(0, qi - 4), j == qi)

                def extra_w(j, lo, hi):
                    ex = []
                    if j >= 4 * c:
                        d0 = (j - 4 * c) * 128
                        ex.append((ident[:], tri[:, 0:128], (d0, d0 + 128)))
                    if j < 4 * c:
                        d0 = (j + 4 - 4 * c) * 128
                        ex.append((ident[:], tri[:, 128:256], (d0, d0 + 128)))
                    return ex
                for h in range(8):
                    po = nxt("o", PSO)
                    tl = [(j, max(0, j - 4 * c), min(3, j + 4 - 4 * c) + 1) for j in range(max(0, 4 * c - 4), 4 * c + 4)]
                    attend(h, tl, lambda j, rows: KW[rows, j * 128:(j + 1) * 128], lambda j, g: VV[:, j, 2 + g, :], [VV], extra_w, po)
                    pend.append((lambda po=po, h=h: finish(po, h, 2, False), (po,)))
                flush()
                for tt in range(4):
                    ub = nxt("u", U)
                    rms_rstd(YATT[:, tt, :], [YATT], 512, 0, junk=ub)
                    fin_rstd(0, 512)
                    sch.op("dve", lambda e, ub=ub, tt=tt: e.tensor_scalar(ub[:, 0:512], YATT[:, tt, :], st1[:, 0:1], None, ALU.mult),
                           reads=[YATT, st1], writes=[ub])
                    transp(ub, 4, 36, MT, tt, kofs=4)
                sch.barrier()

            mark("(c) c%d" % c)
            out_proj_add(MT, wout_d, 0)

            mark("(d) c%d" % c)
            with ExitStack() as sa:
                QX = sb([128, 8, CH], BF16, sa)
                OXA = [sb([128, D], BF16, sa) for _ in range(4)]
                sm = sb([128, 8], F32, sa)
                for tt in range(4):
                    norm_T(H[:, tt, :], [H], 8, UT, tt)
                for u in range(2):
                    wb, w3 = wstream8(wqx_d[:, u * 512:(u + 1) * 512])
                    for j in range(4):
                        ps = proj_fm(lambda k, j=j, w3=w3: w3[:, k, j * 128:(j + 1) * 128], [wb], UT)
                        sch.op("act", lambda e, ps=ps, ct=u * 4 + j: e.activation(out=QX[:, ct, :], in_=ps[:, :], func=AF.Copy),
                               reads=[ps], writes=[QX])
                def x_scores(h):
                    pts = []
                    for mt in range(2):
                        ps = nxt("g", PSG)
                        pt = PT[(2 * h + mt) % 4]

                        def f(e, ps=ps, h=h, mt=mt):
                            for jj in range(2):
                                ins = e.matmul(ps[:, :], KXT[:, 2 * h + jj, mt * 128:(mt + 1) * 128], QX[:, 2 * h + jj, :], start=(jj == 0), stop=(jj == 1))
                            return ins
                        sch.op("pe", f, reads=[KXT, QX], writes=[ps])
                        sch.op("act", lambda e, ps=ps, pt=pt: e.activation(out=pt[:, :], in_=ps[:, :], func=AF.Exp, scale=1.0 / 16),
                               reads=[ps], writes=[pt])
                        pts.append(pt)
                    return pts

                def x_pv(h, pts):
                    for tt in range(4):
                        po = nxt("o", PSO)

                        def f(e, po=po, tt=tt, h=h, pts=pts):
                            for mt in range(2):
                                ins = e.matmul(po[:, 0:257], pts[mt][:, tt * 128:(tt + 1) * 128], VX[:, mt, h, :], start=(mt == 0), stop=(mt == 1))
                            return ins
                        sch.op("pe", f, reads=pts + [VX], writes=[po])
                        sch.op("dve", lambda e, po=po, tt=tt: e.reciprocal(sm[:, tt:tt + 1], po[:, 256:257]), reads=[po], writes=[sm])
                        ox = OXA[tt]
                        sch.op("dve", lambda e, po=po, ox=ox, h=h, tt=tt: e.tensor_scalar(ox[:, h * 256:(h + 1) * 256], po[:, 0:256], sm[:, tt:tt + 1], None, ALU.mult),
                               reads=[po, sm], writes=[ox])

                prev = None
                for h in range(4):
                    pts = x_scores(h)
                    if prev is not None:
                        x_pv(*prev)
                    prev = (h, pts)
                x_pv(*prev)
                for tt in range(4):
                    transp(OXA[tt], 8, None, MT, tt)
                sch.barrier()
            out_proj_add(MT, wox_d, 1)

            mark("(e) c%d" % c)
            with ExitStack() as sa:
                AT = sb([128, 22, CH], BF16, sa)
                ACC = sb([128, 4, D], F32, sa)
                sg = sb([128, CH], F32, sa); tg = sb([128, CH], F32, sa)
                for tt in range(4):
                    norm_T(H[:, tt, :], [H], 16, UT, tt)
                for n in range(11):
                    wb = stream([(lambda b: v3(b, 16, 256)[:, 0:8, :], wgu_d[:, n * 256:(n + 1) * 256].rearrange("(kc p) n -> p kc n", p=128)),
                                 (lambda b: v3(b, 16, 256)[:, 8:16, :], wgu_d[:, DFF + n * 256: DFF + (n + 1) * 256].rearrange("(kc p) n -> p kc n", p=128))])
                    w3 = v3(wb, 16, 256)
                    for j in range(2):
                        pg = proj_fm(lambda k, j=j, w3=w3: w3[:, k, j * 128:(j + 1) * 128], [wb], UT)
                        pu = proj_fm(lambda k, j=j, w3=w3: w3[:, 8 + k, j * 128:(j + 1) * 128], [wb], UT)
                        sch.op("act", lambda e, pg=pg: e.activation(out=sg[:, :], in_=pg[:, :], func=AF.Sigmoid), reads=[pg], writes=[sg])
                        sch.op("dve", lambda e, pg=pg: e.tensor_tensor(tg[:, :], pg[:, :], sg[:, :], ALU.mult), reads=[pg, sg], writes=[tg])
                        sch.op("dve", lambda e, pu=pu, ct=2 * n + j: e.tensor_tensor(AT[:, ct, :], pu[:, :], tg[:, :], ALU.mult),
                               reads=[pu, tg], writes=[AT])
                for u in range(6):
                    nk = 4 if u < 5 else 2
                    wb = stream([(lambda b, nk=nk: v3(b, 4, 1024)[:, 0:nk, :],
                                  wd_d[u * 512: u * 512 + nk * 128, :].rearrange("(kc p) n -> p kc n", p=128))])
                    w3 = v3(wb, 4, 1024)
                    for tt in range(4):
                        for hf in range(2):
                            ps = nxt("g", PSG)

                            def f(e, ps=ps, w3=w3, tt=tt, hf=hf, nk=nk, u=u):
                                for k in range(nk):
                                    ins = e.matmul(ps[:, :], AT[:, 4 * u + k, tt * 128:(tt + 1) * 128], w3[:, k, hf * 512:(hf + 1) * 512],
                                                   start=(k == 0), stop=(k == nk - 1))
                                return ins
                            sch.op("pe", f, reads=[AT, wb], writes=[ps])
                            dst = ACC[:, tt, hf * 512:(hf + 1) * 512]
                            if u == 0:
                                sch.op("act", lambda e, ps=ps, dst=dst: e.activation(out=dst, in_=ps[:, :], func=AF.Copy), reads=[ps], writes=[ACC])
                            else:
                                sch.op("dve", lambda e, ps=ps, dst=dst: e.tensor_tensor(dst, dst, ps[:, :], ALU.add), reads=[ps, ACC], writes=[ACC])
                for tt in range(4):
                    rms_rstd(ACC[:, tt, :], [ACC], D, 2)
                    fin_rstd(2, D)
                    for hf in range(2):
                        sch.op("dve", lambda e, tt=tt, hf=hf: e.scalar_tensor_tensor(
                            out=ftmp[:, :], in0=ACC[:, tt, hf * 512:(hf + 1) * 512], scalar=st1[:, 2:3], in1=gpost[:, 2, hf * 512:(hf + 1) * 512],
                            op0=ALU.mult, op1=ALU.mult), reads=[ACC, st1, gpost], writes=[ftmp])
                        sch.op("dve", lambda e, tt=tt, hf=hf: e.tensor_tensor(
                            H[:, tt, hf * 512:(hf + 1) * 512], H[:, tt, hf * 512:(hf + 1) * 512], ftmp[:, :], ALU.add),
                            reads=[ftmp, H], writes=[H])
                sch.barrier()
            for tt in range(4):
                sch.dma("sp", out_d[c * CH + tt * 128: c * CH + (tt + 1) * 128, :], H[:, tt, :], reads=[H])
        if _LIMIT is not None:
            sch.dma("sp", out_d[0:128, :], H[:, 0, :], reads=[H], force=True)
        sch.final_wait("sp")
    return nc


_NC_CACHE = {}


def _consts(S):
    NCMP = (S - 32) // 16 + 1
    NCT = (NCMP + 127) // 128
    NSLC = S // 64
    p = np.arange(128)
    ident = np.eye(128, dtype=np.float32)
    ewide = (np.arange(S)[None, :] // 64 == np.arange(64)[:, None]).astype(np.float32)
    kk = p[:, None]; tt = np.arange(128)[None, :]
    tri = np.concatenate([np.where(kk > tt, -NEG, 0.0), np.where(kk <= tt, -NEG, 0.0)], axis=1).astype(np.float32)
    cmpb = np.zeros((128, 4 * CH), np.float32)
    for dl in range(4):
        t = np.arange(CH)[None, :]
        cmpb[:, dl * CH:(dl + 1) * CH] = np.where(t + 512 * dl < 16 * kk + 31, -NEG, 0.0)
    pat = np.zeros((128, 256), np.float32)
    crel = (p >= 64).astype(np.int64)[:, None]
    r = np.arange(128)[None, :] - 64
    pat[:, 0:128] = (r < crel - 1).astype(np.float32)
    pat[:, 128:256] = np.where(r == crel - 1, 1.1e4, 0.0) + np.where(r == crel, 1.2e4, 0.0) + np.where(r > crel, -1.0e4, 0.0)
    ci = np.arange(NCMP)[:, None] * 16
    sj = np.arange(NSLC)[None, :] * 64
    ov = np.clip(np.minimum(ci + 32, sj + 64) - np.maximum(ci, sj), 0, None) / 32.0
    maug = np.zeros((128, NCT, 64), np.float32)
    for j in range(NCT):
        n = min(128, NCMP - j * 128)
        maug[:n, j, :NSLC] = ov[j * 128: j * 128 + n]
    cols = np.zeros((128, 4), np.float32)
    cols[:, 0] = (10000.0 ** (-(p % 32).astype(np.float32) / 32.0)).astype(np.float32)
    cols[:, 1] = np.where((p % 64) < 32, -1.0, 1.0)
    cols[:, 2] = EPS
    return dict(ident=ident, ewide=ewide, tri=tri, cmpb=cmpb, pat=pat, maug=maug.reshape(128, NCT * 64), cols=cols)


def _prep_shared(S, inp):
    f = lambda a: np.ascontiguousarray(np.asarray(a, dtype=np.float32))
    w_in = f(inp["w_in"])
    d = np.arange(64); sw = (d + 32) % 64
    ksl = 2304; kw = 2560; kc = 2048; vc = 2176; vsl = 2432; vw = 2688; q0 = 1536
    g2 = lambda base, perm: np.concatenate([base + g * 64 + perm for g in range(2)])
    dup = lambda base, g: np.concatenate([base + g * 64 + d, base + g * 64 + d])
    idxK = np.concatenate([g2(ksl, d), g2(ksl, sw), g2(kw, d), g2(kw, sw), dup(kc, 0), dup(kc, 1), dup(vc, 0), dup(vc, 1),
                           vsl + np.arange(128), vw + np.arange(128)])
    pair = lambda perm: np.concatenate([np.concatenate([q0 + 64 * p + perm, q0 + 64 * (p + 4) + perm]) for p in range(4)])
    idxQ = np.concatenate([pair(d), pair(sw), np.arange(0, 1536)])
    w2k = f(inp["w2_kc"]); w2v = f(inp["w2_vc"])
    gc = lambda v, n: f(v).reshape(n, 128).T
    sh = dict(
        wk=f(w_in[:, idxK]), wq=f(w_in[:, idxQ]), wg=f(w_in[:, 2816:2840]),
        w1k=f(inp["w1_kc"]), w1v=f(inp["w1_vc"]),
        w2k=f(np.concatenate([w2k, w2k, w2k[:, sw], w2k[:, sw]], axis=1)), w2v=w2v,
        pek=f(f(inp["pe_kc"]).reshape(16, 128).T), pev=f(f(inp["pe_vc"]).reshape(16, 128).T),
        cw=f(f(inp["conv_w"]).T.reshape(4, 128, 3).transpose(1, 0, 2).reshape(128, 12)),
        gcols=f(np.concatenate([gc(inp["norm_mix_pre"], 8), gc(inp["norm_x_pre"], 8), gc(inp["norm_ffn_pre"], 8),
                                gc(inp["norm_mem"], 8), gc(inp["norm_conv_out"], 4), gc(inp["norm_attn_out"], 4)], axis=1)),
        gpost=f(np.stack([f(inp["norm_mix_post"]), f(inp["norm_x_post"]), f(inp["norm_ffn_post"])])),
        w_out=f(inp["w_out"]), w_q_x=f(inp["w_q_x"]), w_kv_x=f(inp["w_kv_x"]), w_o_x=f(inp["w_o_x"]),
        w_gate_up=f(inp["w_gate_up"]), w_down=f(inp["w_down"]),
    )
    sh.update(_consts(S))
    return sh


def kernel(**inp):
    x = np.asarray(inp["x"], dtype=np.float32)
    mem = np.asarray(inp["mem"], dtype=np.float32)
    pos = np.asarray(inp["positions"]).astype(np.int32)
    B, S, _ = x.shape
    NCMP = (S - 32) // 16 + 1
    NCP = ((NCMP + 127) // 128) * 128
    if S not in _NC_CACHE:
        _NC_CACHE[S] = build_nc(S)
    nc = _NC_CACHE[S]
    sh = _prep_shared(S, inp)
    in_maps = []
    for b in range(B):
        pc = np.zeros((1, NCP), np.int32)
        pc[0, :NCMP] = pos[b, 31::16][:NCMP]
        m = dict(sh)
        m.update(x=np.ascontiguousarray(x[b]), mem=np.ascontiguousarray(mem[b]), pos=np.ascontiguousarray(pos[b:b + 1]), posc=pc)
        in_maps.append(m)
    res = run_bass_kernel_spmd(nc, in_maps, core_ids=list(range(B)))
    return np.stack([np.asarray(r["out"], dtype=np.float32) for r in res.results], axis=0)
```

```python
import numpy as np
from contextlib import ExitStack
import concourse.bass as bass
import concourse.mybir as mybir
from concourse.bass_utils import run_bass_kernel_spmd

F32 = mybir.dt.float32
BF16 = mybir.dt.bfloat16
I32 = mybir.dt.int32
AF = mybir.ActivationFunctionType
ALU = mybir.AluOpType
AX = mybir.AxisListType

D = 1024
DFF = 2816
MEM = 256
CH = 512
EPS = 1e-6
NEG = 30000.0
PI = float(np.pi)
C1 = 6.28125
C2 = float(2 * np.pi - 6.28125)


_LIMIT = None
_MARKS = []


class Buf:
    def __init__(self, t):
        self.t = t
        self.w = []
        self.r = {}

    def __getitem__(self, k):
        return self.t[k]


class Sch:
    def __init__(self, nc, es, n_dma=32):
        self.nc = nc
        self.E = {"pe": nc.tensor, "act": nc.scalar, "dve": nc.vector, "pool": nc.gpsimd, "sp": nc.sync}
        self.sem = {e: es.enter_context(nc.semaphore("s_" + e)) for e in self.E}
        self.cnt = {e: 0 for e in self.E}
        self.seen = {e: {} for e in self.E}
        self.dsem = [es.enter_context(nc.semaphore("d%d" % i)) for i in range(n_dma)]
        self.duse = [0] * n_dma
        self.qpool = {"pool": list(range(0, n_dma // 2)), "sp": list(range(n_dma // 2, n_dma))}
        self.qnext = {"pool": 0, "sp": 0}

    def _wait(self, eng, tok):
        kind, k, n = tok
        key = (kind, k)
        if self.seen[eng].get(key, 0) >= n:
            return
        if kind == "e":
            self.E[eng].wait_ge(self.sem[k], n)
        else:
            self.E[eng].wait_ge(self.dsem[k], 16 * n)
        self.seen[eng][key] = n

    def _deps(self, eng, reads, writes):
        isdma = eng in ("pool", "sp")
        for b in reads:
            for tok in b.w:
                self._wait(eng, tok)
        for b in writes:
            for tok in b.w:
                if not (isdma and tok[0] == "d"):
                    self._wait(eng, tok)
            for tok in b.r.values():
                self._wait(eng, tok)

    def _mark(self, tok, reads, writes):
        for b in reads:
            b.r[(tok[0], tok[1])] = tok
        for b in writes:
            if tok[0] == "d" and b.w and all(t[0] == "d" for t in b.w) and not b.r:
                b.w = b.w + [tok]
            else:
                b.w = [tok]
            b.r = {}

    def op(self, eng, fn, reads=(), writes=()):
        self.nops = getattr(self, "nops", 0) + 1
        if _LIMIT is not None and self.nops > _LIMIT:
            return
        self._deps(eng, reads, writes)
        inst = fn(self.E[eng])
        self.cnt[eng] += 1
        inst.then_inc(self.sem[eng], 1)
        self._mark(("e", eng, self.cnt[eng]), reads, writes)

    def dma(self, q, out, in_, reads=(), writes=(), force=False):
        self.nops = getattr(self, "nops", 0) + 1
        if _LIMIT is not None and self.nops > _LIMIT and not force:
            return
        pl = self.qpool[q]
        k = pl[self.qnext[q]]
        self.qnext[q] = (self.qnext[q] + 1) % len(pl)
        if self.duse[k] > 0:
            self._wait(q, ("d", k, self.duse[k]))
        self._deps(q, reads, writes)
        self.E[q].dma_start(out=out, in_=in_).then_inc(self.dsem[k], 16)
        self.duse[k] += 1
        self._mark(("d", k, self.duse[k]), reads, writes)

    def barrier(self, engs=("pe", "act", "dve"), dma=False):
        for e in tuple(engs) + (("pool", "sp") if dma else ()):
            for e2 in engs:
                if e2 != e and self.cnt[e2] > 0:
                    self._wait(e, ("e", e2, self.cnt[e2]))

    def final_wait(self, eng):
        for k in range(len(self.dsem)):
            if self.duse[k] > 0:
                self._wait(eng, ("d", k, self.duse[k]))


def build_nc(S):
    NCH = S // CH
    NT = S // 128
    NSLC = S // 64
    NCMP = (S - 32) // 16 + 1
    NCT = (NCMP + 127) // 128
    NCP = NCT * 128
    nc = bass.Bass("TRN2", target_bir_lowering=False)

    def din(name, shape, dt=F32):
        return nc.dram_tensor(name, shape, dt, kind="ExternalInput").ap()

    x_d = din("x", [S, D]); mem_d = din("mem", [MEM, D]); pos_d = din("pos", [1, S], I32)
    posc_d = din("posc", [1, NCP], I32)
    wk_d = din("wk", [D, 1280]); wq_d = din("wq", [D, 2560]); wg_d = din("wg", [D, 24])
    w1k_d = din("w1k", [2048, 256]); w1v_d = din("w1v", [2048, 256])
    w2k_d = din("w2k", [256, 256]); w2v_d = din("w2v", [256, 64])
    pek_d = din("pek", [128, 16]); pev_d = din("pev", [128, 16])
    cw_d = din("cw", [128, 12])
    gcols_d = din("gcols", [128, 40])
    gpost_d = din("gpost", [3, D])
    wout_d = din("w_out", [D, D]); wqx_d = din("w_q_x", [D, D]); wkvx_d = din("w_kv_x", [D, 2 * D])
    wox_d = din("w_o_x", [D, D]); wgu_d = din("w_gate_up", [D, 2 * DFF]); wd_d = din("w_down", [DFF, D])
    ident_d = din("ident", [128, 128]); ewide_d = din("ewide", [64, S]); tri_d = din("tri", [128, 256])
    cmpb_d = din("cmpb", [128, 4 * CH]); pat_d = din("pat", [128, 256]); maug_d = din("maug", [128, NCT * 64])
    cols_d = din("cols", [128, 4])
    out_d = nc.dram_tensor("out", [S, D], F32, kind="ExternalOutput").ap()

    es = ExitStack()
    with es:
        sch = Sch(nc, es)
        uid = [0]

        def sb(shape, dt, stack=es, name=None):
            uid[0] += 1
            return Buf(stack.enter_context(nc.sbuf_tensor((name or "t") + str(uid[0]), shape, dt)))

        def psb(shape, dt):
            uid[0] += 1
            return Buf(es.enter_context(nc.psum_tensor("p" + str(uid[0]), shape, dt)))

        PSG = [psb([128, 512], F32) for _ in range(4)]
        PST = [psb([128, 1024], BF16) for _ in range(2)]
        PSO = [psb([128, 512], F32) for _ in range(2)]
        rot = {"g": 0, "t": 0, "o": 0, "s": 0, "u": 0}

        def nxt(kind, pool):
            rot[kind] = (rot[kind] + 1) % len(pool)
            return pool[rot[kind]]

        ident = sb([128, 128], BF16); ones = sb([128, 128], BF16)
        tri = sb([128, 256], BF16); cmpb = sb([128, 4 * CH], BF16)
        pat = sb([128, 256], F32)
        cols = sb([128, 4], F32); gcols = sb([128, 40], F32); cw = sb([128, 12], F32)
        gpost = sb([128, 3, D], F32)
        WG = sb([128, 8, 24], BF16)
        KE = [sb([128, S], BF16), sb([128, S], BF16)]
        KW = sb([128, S], BF16)
        VV = sb([128, NT, 4, 65], BF16)
        KCMP = sb([128, NCP], BF16); CM = sb([128, NCT, 2, 128], BF16)
        KXT = sb([128, 8, MEM], BF16); VX = sb([128, 2, 4, 257], BF16)
        H = sb([128, 4, D], F32)
        U = [sb([128, D], BF16) for _ in range(2)]
        UT = sb([128, 8, CH], BF16)
        MT = sb([128, 8, CH], BF16)
        QS = sb([128, 8, CH], BF16)
        PT = [sb([128, CH], BF16) for _ in range(4)]
        STR = [sb([128, 4096], BF16) for _ in range(3)]
        st1 = sb([128, 8], F32)
        POSI = sb([128, CH], I32)

        sch.dma("pool", KE[0][64:128, :], ewide_d, writes=[KE[0]])
        sch.dma("pool", KE[1][0:64, :], ewide_d, writes=[KE[1]])
        for g_ in range(2):
            sch.dma("pool", CM[:, :, g_, 64:128], maug_d.rearrange("p (j s) -> p j s", j=NCT), writes=[CM])
        for (dst, src) in ((ident, ident_d), (tri, tri_d), (cmpb, cmpb_d)):
            sch.dma("pool", dst[:], src, writes=[dst])
        for (dst, src) in ((pat, pat_d), (cols, cols_d), (gcols, gcols_d), (cw, cw_d)):
            sch.dma("sp", dst[:], src, writes=[dst])
        for i in range(3):
            sch.dma("sp", gpost[:, i, :], gpost_d[i:i + 1, :].partition_broadcast(128), writes=[gpost])
        sch.dma("pool", WG[:], wg_d.rearrange("(kc p) n -> p kc n", p=128), writes=[WG])
        sch.op("dve", lambda e: e.memset(ones[:], 1.0), writes=[ones])
        sch.op("dve", lambda e: e.memset(VV[:], 1.0), writes=[VV])
        sch.op("dve", lambda e: e.memset(VX[:], 1.0), writes=[VX])
        INV = cols[:, 0:1]; SIGN = cols[:, 1:2]; EPSC = cols[:, 2:3]

        def stream(parts):
            b = nxt("s", STR)
            for vf, src in parts:
                sch.dma("pool", vf(b), src, writes=[b])
            return b

        def v3(b, a, c):
            return b[:, 0:a * c].rearrange("p (a c) -> p a c", a=a)

        def rms_rstd(src_ap, src_bufs, n, col, junk=None):
            junk = junk if junk is not None else nxt("u", U)
            sch.op("dve", lambda e: e.memset(st1[:, col:col + 1], 0.0), writes=[st1])
            sch.op("act", lambda e: e.activation(out=junk[:, 0:src_ap.shape[-1]], in_=src_ap, func=AF.Square,
                                                 accum_out=st1[:, col:col + 1]),
                   reads=src_bufs, writes=[junk, st1])

        def fin_rstd(col, n):
            sch.op("act", lambda e: e.activation(out=st1[:, col:col + 1], in_=st1[:, col:col + 1], func=AF.Sqrt,
                                                 bias=EPSC, scale=1.0 / n), reads=[st1, cols], writes=[st1])
            sch.op("dve", lambda e: e.reciprocal(st1[:, col:col + 1], st1[:, col:col + 1]), reads=[st1], writes=[st1])

        def norm_T(src_ap, src_bufs, gofs, dstT, tt, nk=8):
            ub = nxt("u", U)
            rms_rstd(src_ap, src_bufs, nk * 128, 0, junk=ub)
            fin_rstd(0, nk * 128)
            sch.op("dve", lambda e: e.tensor_scalar(ub[:, 0:nk * 128], src_ap, st1[:, 0:1], None, ALU.mult),
                   reads=src_bufs + [st1], writes=[ub])
            transp(ub, nk, gofs, dstT, tt)

        def transp(ub, nk, gofs, dstT, tt, kofs=0):
            pt = nxt("t", PST)

            def f(e):
                for k in range(nk):
                    ins = e.transpose(pt[:, k * 128:(k + 1) * 128], ub[:, k * 128:(k + 1) * 128], ident[:])
                return ins
            sch.op("pe", f, reads=[ub, ident], writes=[pt])
            dst = dstT[:, kofs:kofs + nk, tt * 128:(tt + 1) * 128]
            src = pt[:, 0:nk * 128].rearrange("p (k c) -> p k c", k=nk)
            if gofs is None:
                sch.op("act", lambda e: e.activation(out=dst, in_=src, func=AF.Copy), reads=[pt], writes=[dstT])
            else:
                g = gcols[:, gofs:gofs + nk].unsqueeze(2).broadcast_to([128, nk, 128])
                sch.op("dve", lambda e: e.tensor_tensor(dst, src, g, ALU.mult), reads=[pt, gcols], writes=[dstT])

        def proj_fm(W_ap_fn, wbufs, rhsT, ncol=CH, nk=8, M=128):
            ps = nxt("g", PSG)

            def f(e):
                for k in range(nk):
                    ins = e.matmul(ps[0:M, 0:ncol], W_ap_fn(k), rhsT[:, k, 0:ncol], start=(k == 0), stop=(k == nk - 1))
                return ins
            sch.op("pe", f, reads=wbufs + [rhsT], writes=[ps])
            return ps

        def tables(stack, posi, n, COS, SINS):
            ang = sb([128, n], F32, stack); ki = sb([128, n], I32, stack); kf = sb([128, n], F32, stack)
            r = sb([128, n], F32, stack)
            sch.op("dve", lambda e: e.tensor_copy(ang[:], posi[:]), reads=[posi], writes=[ang])
            sch.op("dve", lambda e: e.tensor_scalar(ang[:], ang[:], INV, None, ALU.mult), reads=[ang, cols], writes=[ang])
            for shift, dst, sc in ((0.0, SINS, SIGN), (PI / 2, COS, None)):
                sch.op("dve", lambda e, s=shift: e.tensor_scalar(ki[:], ang[:], 1.0 / (2 * PI), s / (2 * PI), ALU.mult, ALU.add),
                       reads=[ang], writes=[ki])
                sch.op("dve", lambda e: e.tensor_copy(kf[:], ki[:]), reads=[ki], writes=[kf])
                sch.op("dve", lambda e: e.scalar_tensor_tensor(out=r[:], in0=kf[:], scalar=-C1, in1=ang[:], op0=ALU.mult, op1=ALU.add),
                       reads=[kf, ang], writes=[r])
                sch.op("dve", lambda e: e.scalar_tensor_tensor(out=r[:], in0=kf[:], scalar=-C2, in1=r[:], op0=ALU.mult, op1=ALU.add),
                       reads=[kf, r], writes=[r])
                sch.op("dve", lambda e, s=shift: e.tensor_scalar(r[:], r[:], s, 3.14159, ALU.add, ALU.min), reads=[r], writes=[r])
                sch.op("dve", lambda e: e.tensor_scalar(r[:], r[:], -3.14159, None, ALU.max), reads=[r], writes=[r])
                if sc is None:
                    sch.op("act", lambda e, d=dst: e.activation(out=d[:], in_=r[:], func=AF.Sin), reads=[r], writes=[dst])
                else:
                    sch.op("act", lambda e, d=dst: e.activation(out=d[:], in_=r[:], func=AF.Sin, scale=SIGN),
                           reads=[r, cols], writes=[dst])

        def rope(stack_tmp, A, Asw, COS, SINS, dst_ap, dst_buf, n, rows=slice(0, 128), cs=None):
            t1, t2 = stack_tmp
            cosap = COS[rows, 0:n] if cs is None else cs(COS)
            sinap = SINS[rows, 0:n] if cs is None else cs(SINS)
            sch.op("dve", lambda e: e.tensor_tensor(t1[rows, 0:n], A[rows, 0:n], cosap, ALU.mult), reads=[A, COS], writes=[t1])
            sch.op("dve", lambda e: e.tensor_tensor(t2[rows, 0:n], Asw[rows, 0:n], sinap, ALU.mult), reads=[Asw, SINS], writes=[t2])
            sch.op("dve", lambda e: e.tensor_tensor(dst_ap, t1[rows, 0:n], t2[rows, 0:n], ALU.add), reads=[t1, t2], writes=[dst_buf])

        def mark(name):
            _MARKS.append((name, getattr(sch, "nops", 0), dict(sch.cnt)))

        def load_x_chunk(c, hb=None):
            hb = H if hb is None else hb
            for tt in range(4):
                sch.dma("sp", hb[:, tt, :], x_d[c * CH + tt * 128: c * CH + (tt + 1) * 128, :], writes=[hb])

        mark("consts_done")
        with ExitStack() as ps0:
            mt_ = sb([128, 2, D], F32, ps0)
            mnT = sb([128, 8, MEM], BF16, ps0)
            for t in range(2):
                sch.dma("sp", mt_[:, t, :], mem_d[t * 128:(t + 1) * 128, :], writes=[mt_])
            for t in range(2):
                norm_T(mt_[:, t, :], [mt_], 24, mnT, t)
            for u in range(2):
                wb = stream([(lambda b: v3(b, 8, 512), wkvx_d[:, u * 512:(u + 1) * 512].rearrange("(kc p) n -> p kc n", p=128))])
                w3 = v3(wb, 8, 512)
                for j in range(4):
                    ps = proj_fm(lambda k, j=j, w3=w3: w3[:, k, j * 128:(j + 1) * 128], [wb], mnT, ncol=MEM)
                    sch.op("act", lambda e, ps=ps, ct=u * 4 + j: e.activation(out=KXT[:, ct, :], in_=ps[:, 0:MEM], func=AF.Copy),
                           reads=[ps], writes=[KXT])
            for u in range(2):
                wb = stream([(lambda b: v3(b, 8, 512), wkvx_d[:, D + u * 512: D + (u + 1) * 512].rearrange("(kc p) n -> p kc n", p=128))])
                w3 = v3(wb, 8, 512)
                for t in range(2):
                    ps = nxt("g", PSG)

                    def f(e, ps=ps, w3=w3, t=t):
                        for k in range(8):
                            ins = e.matmul(ps[:, :], mnT[:, k, t * 128:(t + 1) * 128], w3[:, k, :], start=(k == 0), stop=(k == 7))
                        return ins
                    sch.op("pe", f, reads=[wb, mnT], writes=[ps])
                    sch.op("act", lambda e, ps=ps, t=t, u=u: e.activation(
                        out=VX[:, t, 2 * u:2 * u + 2, 0:256], in_=ps[:, :].rearrange("p (h c) -> p h c", h=2), func=AF.Copy),
                        reads=[ps], writes=[VX])
            sch.barrier(dma=True)

        mark("phase0_done")
        with ExitStack() as pk:
            WK = sb([128, 8, 1280], BF16, pk)
            W1 = [sb([128, 16, 256], BF16, pk) for _ in range(2)]
            W2K = sb([128, 2, 256], BF16, pk); W2V = sb([128, 2, 64], BF16, pk)
            PE_ = sb([128, 32], BF16, pk)
            HB = sb([128, 8], F32, pk)
            HTA = [sb([128, 2, 2, NCP], BF16, pk) for _ in range(2)]
            KCc = [[sb([128, 528], BF16, pk)] for _ in range(4)]
            COS = sb([128, CH], F32, pk); SINS = sb([128, CH], F32, pk)
            posi = POSI
            pci = sb([128, NCP], I32, pk)
            t1 = sb([128, CH], F32, pk); t2 = sb([128, CH], F32, pk)
            sg = sb([128, 32], F32, pk)
            for k in range(8):
                sch.dma("pool", WK[:, k, :], wk_d[k * 128:(k + 1) * 128, :], writes=[WK])
            for i, wd_ in enumerate((w1k_d, w1v_d)):
                for hh in range(2):
                    sch.dma("pool", W1[i][:, hh * 8:(hh + 1) * 8, :],
                            wd_[hh * 1024:(hh + 1) * 1024, :].rearrange("(m p) n -> p m n", p=128), writes=[W1[i]])
            sch.dma("pool", W2K[:], w2k_d.rearrange("(hc p) n -> p hc n", p=128), writes=[W2K])
            sch.dma("pool", W2V[:], w2v_d.rearrange("(hc p) n -> p hc n", p=128), writes=[W2V])
            sch.dma("pool", PE_[:, 0:16], pek_d, writes=[PE_])
            sch.dma("pool", PE_[:, 16:32], pev_d, writes=[PE_])
            for bl in KCc:
                for b in bl:
                    sch.op("dve", lambda e, b=b: e.memset(b[:], 0.0), writes=[b])
            for i in range(2):
                sch.op("dve", lambda e, i=i: e.memset(HTA[i][:], 0.0), writes=[HTA[i]])
            for kv in range(2):
                for hc in range(2):
                    ps = nxt("g", PSG)

                    def f(e, ps=ps, kv=kv, hc=hc):
                        for m in range(16):
                            ins = e.matmul(ps[:, 0:1], W1[kv][:, m, hc * 128:(hc + 1) * 128], PE_[:, kv * 16 + m: kv * 16 + m + 1],
                                           start=(m == 0), stop=(m == 15))
                        return ins
                    sch.op("pe", f, reads=[W1[kv], PE_], writes=[ps])
                    sch.op("act", lambda e, ps=ps, c_=kv * 2 + hc: e.activation(out=HB[:, c_:c_ + 1], in_=ps[:, 0:1], func=AF.Copy),
                           reads=[ps], writes=[HB])

            mark("pk_setup_done")
            for c in range(NCH):
                mark("pk_chunk%d" % c)
                load_x_chunk(c)
                sch.dma("sp", posi[:], pos_d[:, c * CH:(c + 1) * CH].partition_broadcast(128), writes=[posi])
                for tt in range(4):
                    norm_T(H[:, tt, :], [H], 0, UT, tt)
                with ExitStack() as tk:
                    tables(tk, posi, CH, COS, SINS)
                    sch.barrier()
                for kind in (0, 2):
                    A = proj_fm(lambda k, ct=kind: WK[:, k, ct * 128:(ct + 1) * 128], [WK], UT)
                    Asw = proj_fm(lambda k, ct=kind + 1: WK[:, k, ct * 128:(ct + 1) * 128], [WK], UT)
                    if kind == 2:
                        rope((t1, t2), A, Asw, COS, SINS, KW[:, c * CH:(c + 1) * CH], KW, CH)
                    else:
                        for g in range(2):
                            rws = slice(64 * g, 64 * g + 64)
                            rope((t1, t2), A, Asw, COS, SINS, KE[g][rws, c * CH:(c + 1) * CH], KE[g], CH, rows=rws)
                pp = c % 2
                for kv in range(2):
                    for g in range(2):
                        cur = KCc[kv * 2 + g][0]; prev = cur
                        ct = 4 + kv * 2 + g
                        ps = proj_fm(lambda k, ct=ct: WK[:, k, ct * 128:(ct + 1) * 128], [WK], UT)
                        if c > 0:
                            sch.op("dve", lambda e, cur=cur, prev=prev: e.tensor_copy(cur[:, 0:16], prev[:, 512:528]),
                                   reads=[prev], writes=[cur])
                        sch.op("act", lambda e, cur=cur, ps=ps: e.activation(out=cur[0:64, 16:528], in_=ps[0:64, :], func=AF.Copy),
                               reads=[ps], writes=[cur])
                        sch.op("act", lambda e, cur=cur, ps=ps: e.activation(out=cur[64:128, 15:527], in_=ps[64:128, :], func=AF.Copy),
                               reads=[ps], writes=[cur])
                        i0 = 32 * c - 1
                        lo = 1 if c == 0 else 0
                        for hc in range(2):
                            ph = nxt("g", PSG)

                            def f(e, ph=ph, kv=kv, hc=hc, cur=cur):
                                for m in range(16):
                                    rhs = cur[:, 2 * m: 2 * m + 16 * 31 + 1: 16]
                                    ins = e.matmul(ph[:, 0:32], W1[kv][:, m, hc * 128:(hc + 1) * 128], rhs, start=(m == 0), stop=(m == 15))
                                return ins
                            sch.op("pe", f, reads=[W1[kv], cur], writes=[ph])
                            bc = HB[:, kv * 2 + hc: kv * 2 + hc + 1]
                            sch.op("act", lambda e, ph=ph, bc=bc: e.activation(out=sg[:, :], in_=ph[:, 0:32], func=AF.Sigmoid, bias=bc),
                                   reads=[ph, HB], writes=[sg])
                            sch.op("dve", lambda e, ph=ph, bc=bc, kv=kv, hc=hc, g=g, lo=lo, i0=i0: e.scalar_tensor_tensor(
                                out=HTA[kv][:, hc, g, i0 + lo: i0 + 32], in0=ph[:, lo:32], scalar=bc, in1=sg[:, lo:32],
                                op0=ALU.add, op1=ALU.mult), reads=[ph, sg, HB], writes=[HTA[kv]])
                for tt in range(4):
                    ps = nxt("g", PSG)

                    def f(e, ps=ps, tt=tt):
                        for k in range(8):
                            ins = e.matmul(ps[:, 0:256], UT[:, k, tt * 128:(tt + 1) * 128], WK[:, k, 1024:1280], start=(k == 0), stop=(k == 7))
                        return ins
                    sch.op("pe", f, reads=[UT, WK], writes=[ps])
                    sch.op("act", lambda e, ps=ps, ti=c * 4 + tt: e.activation(
                        out=VV[:, ti, :, 0:64], in_=ps[:, 0:256].rearrange("p (a d) -> p a d", a=4), func=AF.Copy),
                        reads=[ps], writes=[VV])
            mark("pk_chunks_done")
            with ExitStack() as tk:
                COSc = sb([128, NCP], F32, tk); SINc = sb([128, NCP], F32, tk)
                sch.dma("sp", pci[:], posc_d.partition_broadcast(128), writes=[pci])
                tables(tk, pci, NCP, COSc, SINc)
                for g in range(2):
                    rows = slice(64 * g, 64 * g + 64)
                    for j in range(NCT):
                        pa = nxt("g", PSG); pb = nxt("g", PSG)
                        for (pp_, off) in ((pa, 0), (pb, 128)):
                            def f(e, pp_=pp_, off=off, g=g, j=j):
                                for hc in range(2):
                                    ins = e.matmul(pp_[:, 0:128], W2K[:, hc, off:off + 128], HTA[0][:, hc, g, j * 128:(j + 1) * 128],
                                                   start=(hc == 0), stop=(hc == 1))
                                return ins
                            sch.op("pe", f, reads=[W2K, HTA[0]], writes=[pp_])
                        rope((t1, t2), pa, pb, COSc, SINc, KCMP[rows, j * 128:(j + 1) * 128], KCMP, 128, rows=rows,
                             cs=lambda T, j=j, rows=rows: T[rows, j * 128:(j + 1) * 128])
                        pv = nxt("g", PSG)

                        def f(e, pv=pv, g=g, j=j):
                            for hc in range(2):
                                ins = e.matmul(pv[:, 0:64], HTA[1][:, hc, g, j * 128:(j + 1) * 128], W2V[:, hc, :], start=(hc == 0), stop=(hc == 1))
                            return ins
                        sch.op("pe", f, reads=[W2V, HTA[1]], writes=[pv])
                        sch.op("act", lambda e, pv=pv, g=g, j=j: e.activation(out=CM[:, j, g, 0:64], in_=pv[:, 0:64], func=AF.Copy),
                               reads=[pv], writes=[CM])
                sch.barrier()
            sch.barrier(dma=True)
        Hb = [H, sb([128, 4, D], F32)]

        def wstream8(src_ap_cols):
            wb = stream([(lambda b, h=h: v3(b, 8, 512)[:, 4 * h:4 * h + 4, :],
                          src_ap_cols[h * 512:(h + 1) * 512, :].rearrange("(kc p) n -> p kc n", p=128)) for h in range(2)])
            return wb, v3(wb, 8, 512)

        def out_proj_add(srcT, w_d, gi):
            halves = [wstream8(w_d[:, hf * 512:(hf + 1) * 512]) for hf in range(2)]
            for tt in range(4):
                pss = []
                for hf in range(2):
                    wb, w3 = halves[hf]
                    ps = nxt("g", PSG)

                    def f(e, ps=ps, w3=w3, tt=tt):
                        for k in range(8):
                            ins = e.matmul(ps[:, :], srcT[:, k, tt * 128:(tt + 1) * 128], w3[:, k, :], start=(k == 0), stop=(k == 7))
                        return ins
                    sch.op("pe", f, reads=[wb, srcT], writes=[ps])
                    rms_rstd(ps[:, :], [ps], D, 2 + hf)
                    pss.append(ps)
                sch.op("dve", lambda e: e.tensor_tensor(st1[:, 2:3], st1[:, 2:3], st1[:, 3:4], ALU.add), reads=[st1], writes=[st1])
                fin_rstd(2, D)
                for hf in range(2):
                    sch.op("dve", lambda e, ps=pss[hf], hf=hf: e.scalar_tensor_tensor(
                        out=ftmp[:, :], in0=ps[:, :], scalar=st1[:, 2:3], in1=gpost[:, gi, hf * 512:(hf + 1) * 512],
                        op0=ALU.mult, op1=ALU.mult), reads=[pss[hf], st1, gpost], writes=[ftmp])
                    sch.op("dve", lambda e, hf=hf, tt=tt: e.tensor_tensor(
                        H[:, tt, hf * 512:(hf + 1) * 512], H[:, tt, hf * 512:(hf + 1) * 512], ftmp[:, :], ALU.add),
                        reads=[ftmp, H], writes=[H])

        ftmp = sb([128, CH], F32)
        G = sb([128, 4, 24], F32)
        ZHIST = sb([128, 4, 2], F32)
        sch.op("dve", lambda e: e.memset(ZHIST[:], 0.0), writes=[ZHIST])

        load_x_chunk(0, Hb[0])
        for c in range(NCH):
            mark("pq_chunk%d" % c)
            H = Hb[c % 2]
            with ExitStack() as sa:
                COS = sb([128, CH], F32, sa); SINS = sb([128, CH], F32, sa); posi = POSI
                t1 = sb([128, CH], F32, sa); t2 = sb([128, CH], F32, sa)
                sch.dma("sp", posi[:], pos_d[:, c * CH:(c + 1) * CH].partition_broadcast(128), writes=[posi])
                for tt in range(4):
                    norm_T(H[:, tt, :], [H], 0, UT, tt)
                with ExitStack() as tk:
                    tables(tk, posi, CH, COS, SINS)
                    sch.barrier()
                for tt in range(4):
                    ps = nxt("g", PSG)

                    def f(e, ps=ps, tt=tt):
                        for k in range(8):
                            ins = e.matmul(ps[:, 0:24], UT[:, k, tt * 128:(tt + 1) * 128], WG[:, k, :], start=(k == 0), stop=(k == 7))
                        return ins
                    sch.op("pe", f, reads=[UT, WG], writes=[ps])
                    sch.op("act", lambda e, ps=ps, tt=tt: e.activation(out=G[:, tt, :], in_=ps[:, 0:24], func=AF.Sigmoid),
                           reads=[ps], writes=[G])
                wqs = [wstream8(wq_d[:, u * 512:(u + 1) * 512]) for u in range(2)]
                for p in range(4):
                    wbA, wA = wqs[0]; wbS, wS = wqs[1]
                    A = proj_fm(lambda k, p=p, wA=wA: wA[:, k, p * 128:(p + 1) * 128], [wbA], UT)
                    Asw = proj_fm(lambda k, p=p, wS=wS: wS[:, k, p * 128:(p + 1) * 128], [wbS], UT)
                    for g in range(2):
                        rws = slice(64 * g, 64 * g + 64)
                        rope((t1, t2), A, Asw, COS, SINS, QS[rws, p + 4 * g, :], QS, CH, rows=rws)
                sch.barrier()
            with ExitStack() as sa:
                Zc = sb([128, 4, 514], F32, sa); Y = sb([128, 4, CH], F32, sa); YSQ = sb([128, 4, CH], BF16, sa)
                xin = sb([128, CH], F32, sa); t1 = sb([128, CH], F32, sa); RB = sb([128, CH], F32, sa)
                wcs = [wstream8(wq_d[:, 1024 + u * 512: 1024 + (u + 1) * 512]) for u in range(3)]
                for ct in range(4):
                    pb = proj_fm(lambda k, ct=ct, w=wcs[0][1]: w[:, k, ct * 128:(ct + 1) * 128], [wcs[0][0]], UT)
                    pc = proj_fm(lambda k, ct=ct, w=wcs[1][1]: w[:, k, ct * 128:(ct + 1) * 128], [wcs[1][0]], UT)
                    px = proj_fm(lambda k, ct=ct, w=wcs[2][1]: w[:, k, ct * 128:(ct + 1) * 128], [wcs[2][0]], UT)
                    sch.op("act", lambda e, px=px: e.activation(out=xin[:, :], in_=px[:, :], func=AF.Copy), reads=[px], writes=[xin])
                    sch.op("dve", lambda e, ct=ct: e.tensor_copy(Zc[:, ct, 0:2], ZHIST[:, ct, :]), reads=[ZHIST], writes=[Zc])
                    sch.op("dve", lambda e, pc=pc, ct=ct: e.tensor_tensor(Zc[:, ct, 2:514], pc[:, :], xin[:, :], ALU.mult),
                           reads=[pc, xin], writes=[Zc])
                    sch.op("dve", lambda e, ct=ct: e.tensor_copy(ZHIST[:, ct, :], Zc[:, ct, 512:514]), reads=[Zc], writes=[ZHIST])
                    sch.op("dve", lambda e, ct=ct: e.tensor_scalar(t1[:, :], Zc[:, ct, 2:514], cw[:, ct * 3 + 2: ct * 3 + 3], None, ALU.mult),
                           reads=[Zc, cw], writes=[t1])
                    for kk in (1, 0):
                        sch.op("dve", lambda e, ct=ct, kk=kk: e.scalar_tensor_tensor(
                            out=t1[:, :], in0=Zc[:, ct, kk:kk + 512], scalar=cw[:, ct * 3 + kk: ct * 3 + kk + 1], in1=t1[:, :],
                            op0=ALU.mult, op1=ALU.add), reads=[Zc, cw, t1], writes=[t1])
                    sch.op("dve", lambda e, pb=pb, ct=ct: e.tensor_tensor(Y[:, ct, :], pb[:, :], t1[:, :], ALU.mult),
                           reads=[pb, t1], writes=[Y])
                    sch.op("act", lambda e, ct=ct: e.activation(out=YSQ[:, ct, :], in_=Y[:, ct, :], func=AF.Square), reads=[Y], writes=[YSQ])
                ps = nxt("g", PSG)

                def f(e, ps=ps):
                    for ct in range(4):
                        ins = e.matmul(ps[:, :], ones[:], YSQ[:, ct, :], start=(ct == 0), stop=(ct == 3))
                    return ins
                sch.op("pe", f, reads=[ones, YSQ], writes=[ps])
                sch.op("act", lambda e, ps=ps: e.activation(out=RB[:, :], in_=ps[:, :], func=AF.Sqrt, bias=EPSC, scale=1.0 / 512),
                       reads=[ps, cols], writes=[RB])
                sch.op("dve", lambda e: e.reciprocal(RB[:, :], RB[:, :]), reads=[RB], writes=[RB])
                for ct in range(4):
                    sch.op("dve", lambda e, ct=ct: e.scalar_tensor_tensor(
                        out=MT[:, ct, :], in0=Y[:, ct, :], scalar=gcols[:, 32 + ct:33 + ct], in1=RB[:, :], op0=ALU.mult, op1=ALU.mult),
                        reads=[Y, RB, gcols], writes=[MT])
                sch.barrier()

            if c + 1 < NCH:
                load_x_chunk(c + 1, Hb[(c + 1) % 2])
            mark("(b) c%d" % c)
            with ExitStack() as sa:
                YATT = sb([128, 4, 512], F32, sa)
                IMP = sb([128, 2, 4, 64], F32, sa)
                imp2 = sb([128, 64], F32, sa); wrk = sb([128, 64], F32, sa); mx = sb([128, 16], F32, sa)
                nbq = sb([128, 128], BF16, sa)
                sm = sb([128, 8], F32, sa)

                def finish(po, h, br, first, ow=65, with_imp=False):
                    g = h // 4
                    for tt in range(4):
                        o = po[:, tt * ow: tt * ow + 64]
                        if with_imp:
                            io = po[:, tt * ow + 64: tt * ow + 128]
                            sch.op("dve", lambda e, io=io: e.tensor_reduce(sm[:, 0:1], io, AX.X, ALU.add), reads=[po], writes=[sm])
                            sch.op("dve", lambda e: e.tensor_scalar(sm[:, 0:1], sm[:, 0:1], 1e-30, None, ALU.max), reads=[sm], writes=[sm])
                        else:
                            dn = po[:, tt * ow + 64: tt * ow + 65]
                            sch.op("dve", lambda e, dn=dn: e.tensor_scalar(sm[:, 0:1], dn, 1e-30, None, ALU.max), reads=[po], writes=[sm])
                        sch.op("dve", lambda e: e.reciprocal(sm[:, 0:1], sm[:, 0:1]), reads=[sm], writes=[sm])
                        sch.op("dve", lambda e, tt=tt: e.tensor_tensor(sm[:, 1:2], sm[:, 0:1], G[:, tt, 3 * h + br: 3 * h + br + 1], ALU.mult),
                               reads=[sm, G], writes=[sm])
                        ya = YATT[:, tt, 64 * h: 64 * h + 64]
                        if first:
                            sch.op("dve", lambda e, o=o, ya=ya: e.tensor_scalar(ya, o, sm[:, 1:2], None, ALU.mult),
                                   reads=[po, sm], writes=[YATT])
                        else:
                            sch.op("dve", lambda e, o=o, ya=ya: e.scalar_tensor_tensor(out=ya, in0=o, scalar=sm[:, 1:2], in1=ya,
                                                                                 op0=ALU.mult, op1=ALU.add),
                                   reads=[po, sm, YATT], writes=[YATT])
                        if with_imp:
                            dst = IMP[:, g, tt, :]
                            if h % 4 == 0:
                                sch.op("dve", lambda e, io=io, dst=dst: e.tensor_scalar(dst, io, sm[:, 0:1], None, ALU.mult),
                                       reads=[po, sm], writes=[IMP])
                            else:
                                sch.op("dve", lambda e, io=io, dst=dst: e.scalar_tensor_tensor(out=dst, in0=io, scalar=sm[:, 0:1], in1=dst,
                                                                                         op0=ALU.mult, op1=ALU.add),
                                       reads=[po, sm, IMP], writes=[IMP])

                def attend(h, tiles, ktile_ap, vtile_ap, vbufs, extra, po, full_k=False, ow=65):
                    g = h // 4
                    rows = slice(64 * g, 64 * g + 64)
                    for pz in [po]:
                        while any(pz in tg for (_, tg) in pend):
                            pend.pop(0)[0]()
                        sch.op("dve", lambda e, pz=pz: e.memset(pz[:, 0:4 * ow], 0.0), writes=[pz])
                    for (j, lo, hi) in tiles:
                        while len(pend) > LOOK:
                            pend.pop(0)[0]()
                        ps = nxt("g", PSG)
                        pt = PT[(rot_pt[0]) % 4]
                        rot_pt[0] += 1
                        cl, chh = lo * 128, hi * 128
                        if full_k:
                            mms = [(ktile_ap(j, rows), QS[:, h, cl:chh], (cl, chh))] + extra(j, lo, hi)
                        else:
                            mms = [(ktile_ap(j, rows), QS[rows, h, cl:chh], (cl, chh))] + extra(j, lo, hi)

                        def f(e, ps=ps, mms=mms):
                            n = len(mms)
                            for i, (l, r, (a, b)) in enumerate(mms):
                                ins = e.matmul(ps[:, a:b], l, r, start=(i == 0), stop=(i == n - 1))
                            return ins
                        sch.op("pe", f, reads=[KE[0], KE[1], KW, KCMP, QS, ident, tri, cmpb], writes=[ps])
                        sch.op("act", lambda e, ps=ps, pt=pt, cl=cl, chh=chh: e.activation(out=pt[:, cl:chh], in_=ps[:, cl:chh], func=AF.Exp,
                                                                                  scale=0.125), reads=[ps], writes=[pt])

                        def f2(e, pt=pt, j=j, lo=lo, hi=hi):
                            for tt in range(lo, hi):
                                ins = e.matmul(po[:, tt * ow: tt * ow + ow], pt[:, tt * 128:(tt + 1) * 128], vtile_ap(j, g),
                                               start=False, stop=False, skip_group_check=True)
                            return ins
                        pend.append((lambda f2=f2, pt=pt: sch.op("pe", f2, reads=[pt] + vbufs, writes=[po]), (po,)))

                def flush():
                    while pend:
                        pend.pop(0)[0]()

                pend = []
                LOOK = 3
                rot_pt = [0]
                jt_c = [j for j in range(NCT) if c - 4 * j >= 0]

                def first_last(j, qi):
                    return (j == jt_c[0], j == jt_c[-1])

                def extra_c(j, lo, hi):
                    dl = c - 4 * j
                    if dl <= 3:
                        return [(ident[:], cmpb[:, dl * CH:(dl + 1) * CH], (0, CH))]
                    return []
                for h in range(8):
                    po = nxt("o", PSO)
                    attend(h, [(j, 0, 4) for j in jt_c], lambda j, rows: KCMP[rows, j * 128:(j + 1) * 128],
                           lambda j, g: CM[:, j, g, :], [CM], extra_c, po, ow=128)
                    pend.append((lambda po=po, h=h: finish(po, h, 0, True, ow=128, with_imp=True), (po,)))
                flush()
                mark("topk c%d" % c)
                for g in range(2):
                    for tt in range(4):
                        qi = 4 * c + tt
                        a0 = 64 - 2 * qi
                        sch.op("dve", lambda e, g=g, tt=tt, a0=a0: e.tensor_tensor(imp2[:, :], IMP[:, g, tt, :], pat[:, a0:a0 + 64], ALU.mult),
                               reads=[IMP, pat], writes=[imp2])
                        sch.op("dve", lambda e, a0=a0: e.tensor_tensor(imp2[:, :], imp2[:, :], pat[:, 128 + a0:128 + a0 + 64], ALU.add),
                               reads=[imp2, pat], writes=[imp2])
                        sch.op("dve", lambda e: e.memset(imp2[:, 0:1], 1.0e4), reads=[imp2], writes=[imp2])
                        sch.op("dve", lambda e: e.max(mx[:, 0:8], imp2[:, :]), reads=[imp2], writes=[mx])
                        sch.op("dve", lambda e: e.match_replace(wrk[:, :], mx[:, 0:8], imp2[:, :], -1e30), reads=[mx, imp2], writes=[wrk])
                        sch.op("dve", lambda e: e.max(mx[:, 8:16], wrk[:, :]), reads=[wrk], writes=[mx])
                        sch.op("dve", lambda e: e.tensor_reduce(sm[:, 2:3], mx[:, 8:16], AX.X, ALU.min), reads=[mx], writes=[sm])
                        for hf in range(2):
                            sch.op("dve", lambda e, hf=hf: e.tensor_scalar(nbq[:, 64 * hf:64 * hf + 64], imp2[:, :], sm[:, 2:3], 1.0, ALU.is_ge, ALU.subtract),
                                   reads=[imp2, sm], writes=[nbq])
                        pt_ = nxt("t", PST)
                        sch.op("pe", lambda e, pt_=pt_: e.transpose(pt_[:, 0:128], nbq[:, :], ident[:]), reads=[nbq, ident], writes=[pt_])
                        mr = slice(64 * (1 - g), 64 * (1 - g) + 64)
                        dst4 = QS[mr, 4 * g:4 * g + 4, tt * 128:(tt + 1) * 128]
                        src4 = pt_[mr, 0:128].unsqueeze(1).broadcast_to([64, 4, 128])
                        sch.op("act", lambda e, dst4=dst4, src4=src4: e.activation(out=dst4, in_=src4, func=AF.Copy, scale=NEG),
                               reads=[pt_], writes=[QS])
                mark("br2 c%d" % c)
                def first_last(j, qi):
                    return (j == 0, j == qi)

                for h in range(8):
                    g = h // 4

                    def extra_s(j, lo, hi, g=g):
                        ex = []
                        if j >= 4 * c:
                            d0 = (j - 4 * c) * 128
                            ex.append((ident[:], tri[:, 0:128], (d0, d0 + 128)))
                        return ex
                    po = nxt("o", PSO)
                    attend(h, [(j, max(0, j - 4 * c), 4) for j in range(4 * c + 4)], lambda j, rows, g=g: KE[g][:, j * 128:(j + 1) * 128],
                           lambda j, g: VV[:, j, g, :], [VV], extra_s, po, full_k=True)
                    pend.append((lambda po=po, h=h: finish(po, h, 1, False), (po,)))
                flush()
                mark("br3 c%d" % c)
                def first_last(j, qi):
                    return (j == max(0, qi - 4), j == qi)

                def extra_w(j, lo, hi):
                    ex = []
                    if j >= 4 * c:
                        d0 = (j - 4 * c) * 128
                        ex.append((ident[:], tri[:, 0:128], (d0, d0 + 128)))
                    if j < 4 * c:
                        d0 = (j + 4 - 4 * c) * 128
                        ex.append((ident[:], tri[:, 128:256], (d0, d0 + 128)))
                    return ex
                for h in range(8):
                    po = nxt("o", PSO)
                    tl = [(j, max(0, j - 4 * c), min(3, j + 4 - 4 * c) + 1) for j in range(max(0, 4 * c - 4), 4 * c + 4)]
                    attend(h, tl, lambda j, rows: KW[rows, j * 128:(j + 1) * 128], lambda j, g: VV[:, j, 2 + g, :], [VV], extra_w, po)
                    pend.append((lambda po=po, h=h: finish(po, h, 2, False), (po,)))
                flush()
                for tt in range(4):
                    ub = nxt("u", U)
                    rms_rstd(YATT[:, tt, :], [YATT], 512, 0, junk=ub)
                    fin_rstd(0, 512)
                    sch.op("dve", lambda e, ub=ub, tt=tt: e.tensor_scalar(ub[:, 0:512], YATT[:, tt, :], st1[:, 0:1], None, ALU.mult),
                           reads=[YATT, st1], writes=[ub])
                    transp(ub, 4, 36, MT, tt, kofs=4)
                sch.barrier()

            mark("(c) c%d" % c)
            out_proj_add(MT, wout_d, 0)

            mark("(d) c%d" % c)
            with ExitStack() as sa:
                QX = sb([128, 8, CH], BF16, sa)
                OXA = [sb([128, D], BF16, sa) for _ in range(4)]
                sm = sb([128, 8], F32, sa)
                for tt in range(4):
                    norm_T(H[:, tt, :], [H], 8, UT, tt)
                for u in range(2):
                    wb, w3 = wstream8(wqx_d[:, u * 512:(u + 1) * 512])
                    for j in range(4):
                        ps = proj_fm(lambda k, j=j, w3=w3: w3[:, k, j * 128:(j + 1) * 128], [wb], UT)
                        sch.op("act", lambda e, ps=ps, ct=u * 4 + j: e.activation(out=QX[:, ct, :], in_=ps[:, :], func=AF.Copy),
                               reads=[ps], writes=[QX])
                def x_scores(h):
                    pts = []
                    for mt in range(2):
                        ps = nxt("g", PSG)
                        pt = PT[(2 * h + mt) % 4]

                        def f(e, ps=ps, h=h, mt=mt):
                            for jj in range(2):
                                ins = e.matmul(ps[:, :], KXT[:, 2 * h + jj, mt * 128:(mt + 1) * 128], QX[:, 2 * h + jj, :], start=(jj == 0), stop=(jj == 1))
                            return ins
                        sch.op("pe", f, reads=[KXT, QX], writes=[ps])
                        sch.op("act", lambda e, ps=ps, pt=pt: e.activation(out=pt[:, :], in_=ps[:, :], func=AF.Exp, scale=1.0 / 16),
                               reads=[ps], writes=[pt])
                        pts.append(pt)
                    return pts

                def x_pv(h, pts):
                    for tt in range(4):
                        po = nxt("o", PSO)

                        def f(e, po=po, tt=tt, h=h, pts=pts):
                            for mt in range(2):
                                ins = e.matmul(po[:, 0:257], pts[mt][:, tt * 128:(tt + 1) * 128], VX[:, mt, h, :], start=(mt == 0), stop=(mt == 1))
                            return ins
                        sch.op("pe", f, reads=pts + [VX], writes=[po])
                        sch.op("dve", lambda e, po=po, tt=tt: e.reciprocal(sm[:, tt:tt + 1], po[:, 256:257]), reads=[po], writes=[sm])
                        ox = OXA[tt]
                        sch.op("dve", lambda e, po=po, ox=ox, h=h, tt=tt: e.tensor_scalar(ox[:, h * 256:(h + 1) * 256], po[:, 0:256], sm[:, tt:tt + 1], None, ALU.mult),
                               reads=[po, sm], writes=[ox])

                prev = None
                for h in range(4):
                    pts = x_scores(h)
                    if prev is not None:
                        x_pv(*prev)
                    prev = (h, pts)
                x_pv(*prev)
                for tt in range(4):
                    transp(OXA[tt], 8, None, MT, tt)
                sch.barrier()
            out_proj_add(MT, wox_d, 1)

            mark("(e) c%d" % c)
            with ExitStack() as sa:
                AT = sb([128, 22, CH], BF16, sa)
                ACC = sb([128, 4, D], F32, sa)
                sg = sb([128, CH], F32, sa); tg = sb([128, CH], F32, sa)
                for tt in range(4):
                    norm_T(H[:, tt, :], [H], 16, UT, tt)
                for n in range(11):
                    wb = stream([(lambda b: v3(b, 16, 256)[:, 0:8, :], wgu_d[:, n * 256:(n + 1) * 256].rearrange("(kc p) n -> p kc n", p=128)),
                                 (lambda b: v3(b, 16, 256)[:, 8:16, :], wgu_d[:, DFF + n * 256: DFF + (n + 1) * 256].rearrange("(kc p) n -> p kc n", p=128))])
                    w3 = v3(wb, 16, 256)
                    for j in range(2):
                        pg = proj_fm(lambda k, j=j, w3=w3: w3[:, k, j * 128:(j + 1) * 128], [wb], UT)
                        pu = proj_fm(lambda k, j=j, w3=w3: w3[:, 8 + k, j * 128:(j + 1) * 128], [wb], UT)
                        sch.op("act", lambda e, pg=pg: e.activation(out=sg[:, :], in_=pg[:, :], func=AF.Sigmoid), reads=[pg], writes=[sg])
                        sch.op("dve", lambda e, pg=pg: e.tensor_tensor(tg[:, :], pg[:, :], sg[:, :], ALU.mult), reads=[pg, sg], writes=[tg])
                        sch.op("dve", lambda e, pu=pu, ct=2 * n + j: e.tensor_tensor(AT[:, ct, :], pu[:, :], tg[:, :], ALU.mult),
                               reads=[pu, tg], writes=[AT])
                for u in range(6):
                    nk = 4 if u < 5 else 2
                    wb = stream([(lambda b, nk=nk: v3(b, 4, 1024)[:, 0:nk, :],
                                  wd_d[u * 512: u * 512 + nk * 128, :].rearrange("(kc p) n -> p kc n", p=128))])
                    w3 = v3(wb, 4, 1024)
                    for tt in range(4):
                        for hf in range(2):
                            ps = nxt("g", PSG)

                            def f(e, ps=ps, w3=w3, tt=tt, hf=hf, nk=nk, u=u):
                                for k in range(nk):
                                    ins = e.matmul(ps[:, :], AT[:, 4 * u + k, tt * 128:(tt + 1) * 128], w3[:, k, hf * 512:(hf + 1) * 512],
                                                   start=(k == 0), stop=(k == nk - 1))
                                return ins
                            sch.op("pe", f, reads=[AT, wb], writes=[ps])
                            dst = ACC[:, tt, hf * 512:(hf + 1) * 512]
                            if u == 0:
                                sch.op("act", lambda e, ps=ps, dst=dst: e.activation(out=dst, in_=ps[:, :], func=AF.Copy), reads=[ps], writes=[ACC])
                            else:
                                sch.op("dve", lambda e, ps=ps, dst=dst: e.tensor_tensor(dst, dst, ps[:, :], ALU.add), reads=[ps, ACC], writes=[ACC])
                for tt in range(4):
                    rms_rstd(ACC[:, tt, :], [ACC], D, 2)
                    fin_rstd(2, D)
                    for hf in range(2):
                        sch.op("dve", lambda e, tt=tt, hf=hf: e.scalar_tensor_tensor(
                            out=ftmp[:, :], in0=ACC[:, tt, hf * 512:(hf + 1) * 512], scalar=st1[:, 2:3], in1=gpost[:, 2, hf * 512:(hf + 1) * 512],
                            op0=ALU.mult, op1=ALU.mult), reads=[ACC, st1, gpost], writes=[ftmp])
                        sch.op("dve", lambda e, tt=tt, hf=hf: e.tensor_tensor(
                            H[:, tt, hf * 512:(hf + 1) * 512], H[:, tt, hf * 512:(hf + 1) * 512], ftmp[:, :], ALU.add),
                            reads=[ftmp, H], writes=[H])
                sch.barrier()
            for tt in range(4):
                sch.dma("sp", out_d[c * CH + tt * 128: c * CH + (tt + 1) * 128, :], H[:, tt, :], reads=[H])
        if _LIMIT is not None:
            sch.dma("sp", out_d[0:128, :], H[:, 0, :], reads=[H], force=True)
        sch.final_wait("sp")
    return nc


_NC_CACHE = {}


def _consts(S):
    NCMP = (S - 32) // 16 + 1
    NCT = (NCMP + 127) // 128
    NSLC = S // 64
    p = np.arange(128)
    ident = np.eye(128, dtype=np.float32)
    ewide = (np.arange(S)[None, :] // 64 == np.arange(64)[:, None]).astype(np.float32)
    kk = p[:, None]; tt = np.arange(128)[None, :]
    tri = np.concatenate([np.where(kk > tt, -NEG, 0.0), np.where(kk <= tt, -NEG, 0.0)], axis=1).astype(np.float32)
    cmpb = np.zeros((128, 4 * CH), np.float32)
    for dl in range(4):
        t = np.arange(CH)[None, :]
        cmpb[:, dl * CH:(dl + 1) * CH] = np.where(t + 512 * dl < 16 * kk + 31, -NEG, 0.0)
    pat = np.zeros((128, 256), np.float32)
    crel = (p >= 64).astype(np.int64)[:, None]
    r = np.arange(128)[None, :] - 64
    pat[:, 0:128] = (r < crel - 1).astype(np.float32)
    pat[:, 128:256] = np.where(r == crel - 1, 1.1e4, 0.0) + np.where(r == crel, 1.2e4, 0.0) + np.where(r > crel, -1.0e4, 0.0)
    ci = np.arange(NCMP)[:, None] * 16
    sj = np.arange(NSLC)[None, :] * 64
    ov = np.clip(np.minimum(ci + 32, sj + 64) - np.maximum(ci, sj), 0, None) / 32.0
    maug = np.zeros((128, NCT, 64), np.float32)
    for j in range(NCT):
        n = min(128, NCMP - j * 128)
        maug[:n, j, :NSLC] = ov[j * 128: j * 128 + n]
    cols = np.zeros((128, 4), np.float32)
    cols[:, 0] = (10000.0 ** (-(p % 32).astype(np.float32) / 32.0)).astype(np.float32)
    cols[:, 1] = np.where((p % 64) < 32, -1.0, 1.0)
    cols[:, 2] = EPS
    return dict(ident=ident, ewide=ewide, tri=tri, cmpb=cmpb, pat=pat, maug=maug.reshape(128, NCT * 64), cols=cols)


def _prep_shared(S, inp):
    f = lambda a: np.ascontiguousarray(np.asarray(a, dtype=np.float32))
    w_in = f(inp["w_in"])
    d = np.arange(64); sw = (d + 32) % 64
    ksl = 2304; kw = 2560; kc = 2048; vc = 2176; vsl = 2432; vw = 2688; q0 = 1536
    g2 = lambda base, perm: np.concatenate([base + g * 64 + perm for g in range(2)])
    dup = lambda base, g: np.concatenate([base + g * 64 + d, base + g * 64 + d])
    idxK = np.concatenate([g2(ksl, d), g2(ksl, sw), g2(kw, d), g2(kw, sw), dup(kc, 0), dup(kc, 1), dup(vc, 0), dup(vc, 1),
                           vsl + np.arange(128), vw + np.arange(128)])
    pair = lambda perm: np.concatenate([np.concatenate([q0 + 64 * p + perm, q0 + 64 * (p + 4) + perm]) for p in range(4)])
    idxQ = np.concatenate([pair(d), pair(sw), np.arange(0, 1536)])
    w2k = f(inp["w2_kc"]); w2v = f(inp["w2_vc"])
    gc = lambda v, n: f(v).reshape(n, 128).T
    sh = dict(
        wk=f(w_in[:, idxK]), wq=f(w_in[:, idxQ]), wg=f(w_in[:, 2816:2840]),
        w1k=f(inp["w1_kc"]), w1v=f(inp["w1_vc"]),
        w2k=f(np.concatenate([w2k, w2k, w2k[:, sw], w2k[:, sw]], axis=1)), w2v=w2v,
        pek=f(f(inp["pe_kc"]).reshape(16, 128).T), pev=f(f(inp["pe_vc"]).reshape(16, 128).T),
        cw=f(f(inp["conv_w"]).T.reshape(4, 128, 3).transpose(1, 0, 2).reshape(128, 12)),
        gcols=f(np.concatenate([gc(inp["norm_mix_pre"], 8), gc(inp["norm_x_pre"], 8), gc(inp["norm_ffn_pre"], 8),
                                gc(inp["norm_mem"], 8), gc(inp["norm_conv_out"], 4), gc(inp["norm_attn_out"], 4)], axis=1)),
        gpost=f(np.stack([f(inp["norm_mix_post"]), f(inp["norm_x_post"]), f(inp["norm_ffn_post"])])),
        w_out=f(inp["w_out"]), w_q_x=f(inp["w_q_x"]), w_kv_x=f(inp["w_kv_x"]), w_o_x=f(inp["w_o_x"]),
        w_gate_up=f(inp["w_gate_up"]), w_down=f(inp["w_down"]),
    )
    sh.update(_consts(S))
    return sh


def kernel(**inp):
    x = np.asarray(inp["x"], dtype=np.float32)
    mem = np.asarray(inp["mem"], dtype=np.float32)
    pos = np.asarray(inp["positions"]).astype(np.int32)
    B, S, _ = x.shape
    NCMP = (S - 32) // 16 + 1
    NCP = ((NCMP + 127) // 128) * 128
    if S not in _NC_CACHE:
        _NC_CACHE[S] = build_nc(S)
    nc = _NC_CACHE[S]
    sh = _prep_shared(S, inp)
    in_maps = []
    for b in range(B):
        pc = np.zeros((1, NCP), np.int32)
        pc[0, :NCMP] = pos[b, 31::16][:NCMP]
        m = dict(sh)
        m.update(x=np.ascontiguousarray(x[b]), mem=np.ascontiguousarray(mem[b]), pos=np.ascontiguousarray(pos[b:b + 1]), posc=pc)
        in_maps.append(m)
    res = run_bass_kernel_spmd(nc, in_maps, core_ids=list(range(B)))
    return np.stack([np.asarray(r["out"], dtype=np.float32) for r in res.results], axis=0)
```
